# Optimizing a Trainium2 kernel written in Bass

```python
import jax, jax.numpy as jnp
from jax import lax
import numpy as np

D_MODEL = 1024
BATCH = 16
SEQ = 2048
DEPTH = 1

N_META = 16
POOL_WIDTH = 512
POOL_WINDOWS = (2, 4, 8, 16)
POOL_GROUP = POOL_WIDTH // len(POOL_WINDOWS)
HEAD_DIM = 64
N_HEADS = 8
N_KV_HEADS = 2
Q_PER_KV = N_HEADS // N_KV_HEADS
ATTN_WIDTH = N_HEADS * HEAD_DIM
KV_WIDTH = N_KV_HEADS * HEAD_DIM
WINDOW = 128
BLOCK = 128
ROT_DIM = HEAD_DIM // 4
ROPE_THETA = 500000.0
MIX_WIDTH = POOL_WIDTH + ATTN_WIDTH
IN_WIDTH = POOL_WIDTH + ATTN_WIDTH + 2 * KV_WIDTH
N_GROUPS = 4
EXPERTS_PER_GROUP = 4
N_EXPERTS = N_GROUPS * EXPERTS_PER_GROUP
TOP_K = 2
D_EXPERT = 256
EPS = 1e-6
NEG_INF = -1e30

kernel_name = "hymba_pool_swa_sink_hmoe"


def rms_norm(x, gain):
    xf = x.astype(jnp.float32)
    y = xf * lax.rsqrt(jnp.mean(xf * xf, axis=-1, keepdims=True) + EPS)
    return (y * gain.astype(jnp.float32)).astype(x.dtype)


def partial_rope(t, pos):
    half = ROT_DIM // 2
    inv_freq = 1.0 / (ROPE_THETA ** (jnp.arange(half, dtype=jnp.float32) / half))
    ang = pos.astype(jnp.float32)[:, None] * inv_freq[None, :]
    cos = jnp.cos(ang)[:, None, :]
    sin = jnp.sin(ang)[:, None, :]
    tf = t.astype(jnp.float32)
    x1 = tf[..., :half]
    x2 = tf[..., half:ROT_DIM]
    out = jnp.concatenate([x1 * cos - x2 * sin, x2 * cos + x1 * sin, tf[..., ROT_DIM:]], axis=-1)
    return out.astype(t.dtype)


def multiscale_pool(u, w_pool, pool_scale):
    L = u.shape[1]
    pos = jnp.arange(L)
    outs = []
    for g, w in enumerate(POOL_WINDOWS):
        ug = u[..., g * POOL_GROUP:(g + 1) * POOL_GROUP].astype(jnp.float32)
        c = jnp.cumsum(ug, axis=1)
        c_prev = jnp.pad(c, ((0, 0), (w, 0), (0, 0)))[:, :L]
        count = jnp.minimum(pos + 1, w).astype(jnp.float32)[None, :, None]
        mixed = ((c - c_prev) / count - ug).astype(u.dtype)
        outs.append(jnp.einsum('blc,cd->bld', mixed, w_pool[g]))
    return jnp.concatenate(outs, axis=-1) * pool_scale


def sliding_window_attention(q, k, v, sinks):
    B, L = q.shape[0], q.shape[1]
    nb = -(-L // BLOCK)
    Lp = nb * BLOCK
    pad = ((0, 0), (0, Lp - L), (0, 0), (0, 0))
    qb = jnp.pad(q, pad).reshape(B, nb, BLOCK, N_KV_HEADS, Q_PER_KV, HEAD_DIM)
    kb = jnp.pad(k, pad).reshape(B, nb, BLOCK, N_KV_HEADS, HEAD_DIM)
    vb = jnp.pad(v, pad).reshape(B, nb, BLOCK, N_KV_HEADS, HEAD_DIM)
    shift = ((0, 0), (1, 0), (0, 0), (0, 0), (0, 0))
    k_band = jnp.concatenate([jnp.pad(kb, shift)[:, :nb], kb], axis=2)
    v_band = jnp.concatenate([jnp.pad(vb, shift)[:, :nb], vb], axis=2)
    k_meta = k[:, :N_META]
    v_meta = v[:, :N_META]
    scale = HEAD_DIM ** -0.5
    s_band = jnp.einsum('bnqkgd,bnjkd->bnkgqj', qb, k_band).astype(jnp.float32) * scale
    s_meta = jnp.einsum('bnqkgd,bmkd->bnkgqm', qb, k_meta).astype(jnp.float32) * scale
    q_pos = jnp.arange(nb)[:, None] * BLOCK + jnp.arange(BLOCK)[None, :]
    k_pos = jnp.arange(nb)[:, None] * BLOCK - BLOCK + jnp.arange(2 * BLOCK)[None, :]
    diff = q_pos[:, :, None] - k_pos[:, None, :]
    band_ok = (diff >= 0) & (diff < WINDOW) & (k_pos[:, None, :] >= 0)
    meta_ok = (q_pos[:, :, None] - jnp.arange(N_META)[None, None, :]) >= WINDOW
    s_band = jnp.where(band_ok[None, :, None, None], s_band, NEG_INF)
    s_meta = jnp.where(meta_ok[None, :, None, None], s_meta, NEG_INF)
    sink = jnp.broadcast_to(
        sinks.astype(jnp.float32).reshape(N_KV_HEADS, Q_PER_KV)[None, None, :, :, None, None],
        s_band.shape[:-1] + (1,))
    p = jax.nn.softmax(jnp.concatenate([s_band, s_meta, sink], axis=-1), axis=-1)
    p_band = p[..., :2 * BLOCK].astype(v.dtype)
    p_meta = p[..., 2 * BLOCK:2 * BLOCK + N_META].astype(v.dtype)
    o = (jnp.einsum('bnkgqj,bnjkd->bnqkgd', p_band, v_band)
         + jnp.einsum('bnkgqm,bmkd->bnqkgd', p_meta, v_meta))
    return o.reshape(B, Lp, ATTN_WIDTH)[:, :L]


def hierarchical_moe(x, w_group_router, w_expert_router, w_gate, w_up, w_down):
    B, L, D = x.shape
    t = x.reshape(B * L, D)
    group_probs = jax.nn.softmax((t @ w_group_router).astype(jnp.float32), axis=-1)
    g_prob, g_idx = lax.top_k(group_probs, 1)
    expert_logits = (t @ w_expert_router).astype(jnp.float32).reshape(-1, N_GROUPS, EXPERTS_PER_GROUP)
    g_onehot = jax.nn.one_hot(g_idx[:, 0], N_GROUPS, dtype=jnp.float32)
    in_group = jnp.einsum('tge,tg->te', expert_logits, g_onehot)
    e_logit, e_idx = lax.top_k(in_group, TOP_K)
    e_w = jax.nn.softmax(e_logit, axis=-1) * g_prob
    expert_id = g_idx * EXPERTS_PER_GROUP + e_idx
    gates = jnp.sum(jax.nn.one_hot(expert_id, N_EXPERTS, dtype=jnp.float32) * e_w[..., None], axis=1)
    hid = jax.nn.silu(jnp.einsum('td,edf->tef', t, w_gate)) * jnp.einsum('td,edf->tef', t, w_up)
    hid = hid * gates.astype(hid.dtype)[..., None]
    y = jnp.einsum('tef,efd->td', hid, w_down)
    return y.reshape(B, L, D)


def setup_inputs(seed: int = 0) -> dict:
    key = jax.random.key(seed)
    ks = jax.random.split(key, 20)
    f32 = jnp.float32
    n = lambda k, shape, s: jax.random.normal(k, shape, f32) * s
    return {
        "x": n(ks[0], (BATCH, SEQ, D_MODEL), 1.0),
        "meta_tokens": n(ks[1], (N_META, D_MODEL), 1.0),
        "attn_norm_gain": 1.0 + n(ks[2], (DEPTH, D_MODEL), 0.02),
        "w_in": n(ks[3], (DEPTH, D_MODEL, IN_WIDTH), D_MODEL ** -0.5),
        "w_pool": n(ks[4], (DEPTH, len(POOL_WINDOWS), POOL_GROUP, POOL_GROUP), POOL_GROUP ** -0.5),
        "pool_scale": 1.0 + n(ks[5], (DEPTH, POOL_WIDTH), 0.02),
        "q_norm_gain": 1.0 + n(ks[6], (DEPTH, HEAD_DIM), 0.02),
        "k_norm_gain": 1.0 + n(ks[7], (DEPTH, HEAD_DIM), 0.02),
        "attn_sinks": n(ks[8], (DEPTH, N_HEADS), 0.5),
        "w_out": n(ks[9], (DEPTH, MIX_WIDTH, D_MODEL), MIX_WIDTH ** -0.5),
        "ffn_norm_gain": 1.0 + n(ks[10], (DEPTH, D_MODEL), 0.02),
        "w_group_router": n(ks[11], (DEPTH, D_MODEL, N_GROUPS), D_MODEL ** -0.5),
        "w_expert_router": n(ks[12], (DEPTH, D_MODEL, N_EXPERTS), D_MODEL ** -0.5),
        "w_gate": n(ks[13], (DEPTH, N_EXPERTS, D_MODEL, D_EXPERT), D_MODEL ** -0.5),
        "w_up": n(ks[14], (DEPTH, N_EXPERTS, D_MODEL, D_EXPERT), D_MODEL ** -0.5),
        "w_down": n(ks[15], (DEPTH, N_EXPERTS, D_EXPERT, D_MODEL), D_EXPERT ** -0.5),
    }


def reference(x, meta_tokens, attn_norm_gain, w_in, w_pool, pool_scale, q_norm_gain, k_norm_gain,
              attn_sinks, w_out, ffn_norm_gain, w_group_router, w_expert_router, w_gate, w_up, w_down):
    B, S, D = x.shape
    meta = jnp.broadcast_to(meta_tokens[None].astype(x.dtype), (B, N_META, D))
    h = jnp.concatenate([meta, x], axis=1)
    L = S + N_META
    pos = jnp.arange(L)
    splits = [POOL_WIDTH, POOL_WIDTH + ATTN_WIDTH, POOL_WIDTH + ATTN_WIDTH + KV_WIDTH]
    for layer in range(DEPTH):
        a = rms_norm(h, attn_norm_gain[layer])
        proj = a @ w_in[layer]
        u, q, k, v = jnp.split(proj, splits, axis=-1)
        y_pool = multiscale_pool(u, w_pool[layer], pool_scale[layer])
        q = partial_rope(rms_norm(q.reshape(B, L, N_HEADS, HEAD_DIM), q_norm_gain[layer]), pos)
        k = partial_rope(rms_norm(k.reshape(B, L, N_KV_HEADS, HEAD_DIM), k_norm_gain[layer]), pos)
        v = v.reshape(B, L, N_KV_HEADS, HEAD_DIM)
        y_attn = sliding_window_attention(q, k, v, attn_sinks[layer])
        h = h + jnp.concatenate([y_pool, y_attn], axis=-1) @ w_out[layer]
        m = rms_norm(h, ffn_norm_gain[layer])
        h = h + hierarchical_moe(m, w_group_router[layer], w_expert_router[layer],
                                 w_gate[layer], w_up[layer], w_down[layer])
    return h[:, N_META:]
```

```python
import math
import contextlib
import numpy as np
import concourse.bass as bass
import concourse.mybir as mybir
from concourse.bass_utils import run_bass_kernel_spmd

F32 = mybir.dt.float32
BF16 = mybir.dt.bfloat16
I32 = mybir.dt.int32
ALU = mybir.AluOpType
AF = mybir.ActivationFunctionType
AX = mybir.AxisListType

D = 1024
SEQ = 2048
NSEQ = 2
NT = 32
TB = NT * 128
NR = NSEQ * SEQ // TB
NE = 16
NTT = NSEQ * SEQ // 128
NU = 48
NSLOT = NU * 256
WROW = 8 * 512 + 2 * 1024
POOLW = (2, 4, 8, 16)
EPS = 1e-6
THETA = 500000.0
INV_FREQ = [1.0 / (THETA ** (i / 8.0)) for i in range(8)]


class Op:
    __slots__ = ("eng", "fn", "r", "w", "dma", "deps", "sig", "sigval", "blk")


class Tracker:
    def __init__(self, nc, stack):
        self.nc = nc
        self.stack = stack
        self.ops = []
        self.lastw = {}
        self.readers = {}
        self.sems = {}
        self.cnt = {}
        self.waited = {}
        self.blk = 0
        self.cur = None
        self.noseg = False
        self.step_ops = {}

    def seg(self, slot, stage):
        if not self.noseg:
            self.cur = (slot, stage)

    def end_step(self):
        so = self.step_ops
        self.step_ops = {}
        self.cur = None
        for slot in sorted({k[0] for k in so}):
            items = []
            for k in sorted(so):
                if k[0] != slot:
                    continue
                lst = so[k]
                for i, o in enumerate(lst):
                    items.append(((i + 0.5) / len(lst), k[1], i, o))
            items.sort(key=lambda x: (x[0], x[1], x[2]))
            self.ops.extend(o for _, _, _, o in items)

    def sem(self, key):
        if key not in self.sems:
            self.sems[key] = self.stack.enter_context(self.nc.semaphore("s_" + key))
            self.cnt[key] = 0
        return self.sems[key]

    def op(self, eng, fn, r=(), w=(), dma=None):
        o = Op()
        o.eng, o.fn, o.r, o.w, o.dma = eng, fn, tuple(r), tuple(w), dma
        o.deps, o.sig, o.sigval, o.blk = [], dma is not None, 0, self.blk
        if self.cur is not None:
            self.step_ops.setdefault(self.cur, []).append(o)
        else:
            self.ops.append(o)
        return o

    def flush(self):
        ops, self.ops = self.ops, []
        if not ops:
            return
        for o in ops:
            deps = []
            for b in o.r:
                lw = self.lastw.get(b)
                if lw is not None:
                    deps.append(lw)
            for b in o.w:
                lw = self.lastw.get(b)
                if lw is not None:
                    deps.append(lw)
                deps.extend(self.readers.get(b, ()))
            for b in o.r:
                self.readers.setdefault(b, []).append(o)
            for b in o.w:
                self.lastw[b] = o
                self.readers[b] = []
            seen = set()
            for d in deps:
                if d is o or id(d) in seen or d.blk != self.blk:
                    continue
                seen.add(id(d))
                if d.dma is None and o.dma is None and d.eng == "pe" and o.eng == "pe":
                    continue
                o.deps.append(d)
                d.sig = True
        for o in ops:
            if o.sig:
                key = o.dma or o.eng
                self.sem(key)
                self.cnt[key] += 16 if o.dma else 1
                o.sigval = self.cnt[key]
        engs = []
        for o in ops:
            if o.eng not in engs:
                engs.append(o.eng)
        reg = {"pe": "tensor", "act": "scalar", "dve": "vector", "pool": "gpsimd", "sp": "sync"}
        with self.nc.Block() as blk:
            for eng in engs:
                def body(e, eng=eng):
                    dmakeys = []
                    for o in ops:
                        if o.eng != eng:
                            continue
                        for d in o.deps:
                            key = d.dma or d.eng
                            if self.waited.get((eng, key), 0) < d.sigval:
                                e.wait_ge(self.sems[key], d.sigval)
                                self.waited[(eng, key)] = d.sigval
                        ins = o.fn(e)
                        if o.sig:
                            ins.then_inc(self.sems[o.dma or o.eng], 16 if o.dma else 1)
                        if o.dma and o.dma not in dmakeys:
                            dmakeys.append(o.dma)
                    for key in dmakeys:
                        if self.waited.get((eng, key), 0) < self.cnt[key]:
                            e.wait_ge(self.sems[key], self.cnt[key])
                            self.waited[(eng, key)] = self.cnt[key]
                getattr(blk, reg[eng])(body)
        self.blk += 1


def build_nc():
    nc = bass.Bass("TRN2", target_bir_lowering=False)
    dr = {}

    def din(name, shape):
        dr[name] = nc.dram_tensor(name, list(shape), F32, kind="ExternalInput").ap()

    din("x", (NSEQ * SEQ, D))
    din("meta", (16, D))
    din("ag", (1, D))
    din("w_in", (D, 1280))
    din("w_pool", (4, 128, 128))
    din("pscale", (1, 512))
    din("qg", (1, 64))
    din("kg", (1, 64))
    din("sinks", (1, 8))
    din("w_out", (D, D))
    din("fg", (1, D))
    din("wgr", (D, 4))
    din("wer", (D, 16))
    din("w_gate", (NE, D, 256))
    din("w_up", (NE, D, 256))
    din("w_down", (NE, 256, D))
    out = nc.dram_tensor("out", [NSEQ * SEQ, D], F32, kind="ExternalOutput").ap()
    mscr = nc.dram_tensor("mscr", [NSEQ * SEQ, D], BF16, kind="Internal").ap()
    mslot = nc.dram_tensor("mslot", [NSLOT, D], BF16, kind="Internal").ap()
    yscr = nc.dram_tensor("yscr", [NSLOT, D], F32, kind="Internal").ap()
    wscr = nc.dram_tensor("wscr", [NE * 128, WROW], BF16, kind="Internal").ap()

    with contextlib.ExitStack() as st:
        T = Tracker(nc, st)

        def sb(name, shape, dt=F32):
            return st.enter_context(nc.sbuf_tensor(name, list(shape), dt))

        win = sb("win", [128, 8, 1280], BF16)
        wop = sb("wop", [128, 4, 1024], BF16)
        woa = sb("woa", [128, 4, 1024], BF16)
        wpl = sb("wpl", [128, 4, 128], BF16)
        wr = sb("wr", [128, 8, 20], BF16)
        ag_t = sb("ag_t", [128, 8])
        fg_t = sb("fg_t", [128, 8])
        fgb = sb("fgb", [128, 1024])
        ps_t = sb("ps_t", [128, 4])
        qkgain = sb("qkgain", [128, 10, 64])
        ident = sb("ident", [128, 128], BF16)
        mask_d = sb("mask_d", [128, 128], BF16)
        mask_p = sb("mask_p", [128, 128], BF16)
        negm_d = sb("negm_d", [128, 4, 128], BF16)
        negm_p = sb("negm_p", [128, 4, 128], BF16)
        Pd = sb("Pd", [128, 4, 128], BF16)
        Pp = sb("Pp", [128, 4, 128], BF16)
        Pm = sb("Pm", [16, 4, 128], BF16)
        ones64 = sb("ones64", [128, 64], BF16)
        onesm = sb("onesm", [33, 64], BF16)
        sel = sb("sel", [16, 16, 128], BF16)
        eps_t = sb("eps_t", [128, 1])
        negC = sb("negC", [128, 1])
        cosT = sb("cosT", [128, 17, 8])
        sinT = sb("sinT", [128, 17, 8])
        um = sb("um", [128, 512], BF16)
        KTm = sb("KTm", [128, 128], BF16)
        Vm = [sb("Vm%d" % g, [33, 64], BF16) for g in range(2)]
        PTm = [[sb("PTm%d_%d" % (g, s), [33, 512], BF16) for s in range(2)] for g in range(2)]
        uring = [sb("ur%d" % i, [128, 512], BF16) for i in range(3)]
        vring = [sb("vr%d" % i, [128, 128], BF16) for i in range(3)]
        kring = [sb("kr%d" % i, [128, 128], BF16) for i in range(3)]
        tri = sb("tri", [128, 128], BF16)
        zt = sb("zt", [128, 1024], BF16)
        ones128 = sb("ones128", [128, 128], BF16)
        E1all = sb("E1all", [128, NTT, 16])
        E2all = sb("E2all", [128, NTT, 16])
        Acum = sb("Acum", [128, NTT + 1, 16], BF16)
        Aall = sb("Aall", [128, NTT, 16], BF16)
        w12 = sb("w12", [128, NTT, 2])
        slot1i = sb("slot1i", [128, NTT], I32)
        slot2i = sb("slot2i", [128, NTT], I32)
        widx = sb("widx", [128, NU], I32)

        with contextlib.ExitStack() as s1:
            def sb1(name, shape, dt=F32):
                return s1.enter_context(nc.sbuf_tensor(name, list(shape), dt))

            def ps1(name, shape, dt=F32):
                return s1.enter_context(nc.psum_tensor(name, list(shape), dt))

            stg = [sb1("stg%d" % i, [128, 1280]) for i in range(4)]
            qg_b = sb1("qg_b", [128, 64])
            kg_b = sb1("kg_b", [128, 64])
            sk = sb1("sk", [128, 8])
            es = sb1("es", [128, 8])
            identf = sb1("identf", [128, 128])
            Pf = sb1("Pf", [128, 12, 128])
            posi = sb1("posi", [128, 17], I32)
            posf = sb1("posf", [128, 17])
            ang = sb1("ang", [128, 17, 8])
            angi = sb1("angi", [128, 17, 8], I32)
            angf = sb1("angf", [128, 17, 8])
            tA = sb1("tA", [128, 17, 8])
            tB = sb1("tB", [128, 17, 8])
            mq = sb1("mq", [128, 1])
            mk = sb1("mk", [128, 1])
            wrs = sb1("wrs", [128, 8, 20])

            def ld(dst, src, key, buf, **kw):
                T.op("sp", lambda e: e.dma_start(out=dst, in_=src, **kw), w=[buf], dma=key)

            ld(ag_t[:], dr["ag"].rearrange("o (c p) -> p (o c)", p=128), "L_ag", "ag_t", allow_slow_non_contiguous=True)
            ld(fg_t[:], dr["fg"].rearrange("o (c p) -> p (o c)", p=128), "L_fg", "fg_t", allow_slow_non_contiguous=True)
            ld(ps_t[:], dr["pscale"].rearrange("o (g d) -> d (o g)", d=128), "L_ps", "ps_t", allow_slow_non_contiguous=True)
            ld(qg_b[:], dr["qg"].partition_broadcast(128), "L_qg", "qg_b")
            ld(fgb[:], dr["fg"].partition_broadcast(128), "L_fgb", "fgb")
            ld(kg_b[:], dr["kg"].partition_broadcast(128), "L_kg", "kg_b")
            ld(sk[32:33, :], dr["sinks"], "L_sk", "sk")
            ld(wrs[:, :, 0:4], dr["wgr"].rearrange("(c p) n -> p c n", p=128), "L_wgr", "wrs_a")
            ld(wrs[:, :, 4:20], dr["wer"].rearrange("(c p) n -> p c n", p=128), "L_wer", "wrs_b")

            sidx = [0]
            NSTG = 4

            def stage_load(parts, consume):
                s_ = sidx[0] % NSTG
                sidx[0] += 1
                key = "stg%d" % s_
                for dst_fn, src_ap in parts:
                    T.op("sp", lambda e, dst_fn=dst_fn, src_ap=src_ap, s_=s_: e.dma_start(out=dst_fn(stg[s_]), in_=src_ap),
                         w=[key], dma="L_%s_%d" % (key, len(parts)))
                consume(stg[s_], key)

            for c in range(8):
                def cons(t, key, c=c):
                    T.op("act", lambda e: e.activation(out=win[:, c, :], in_=t[:, 0:1280], func=AF.Copy, scale=ag_t[:, c:c + 1]),
                         r=[key, "ag_t"], w=["win%d" % c])
                stage_load([(lambda t: t[:, 0:1280], dr["w_in"][c * 128:(c + 1) * 128, :])], cons)
            for c in range(4):
                def cons(t, key, c=c):
                    T.op("act", lambda e: e.activation(out=wop[:, c, :], in_=t[:, 0:1024], func=AF.Copy), r=[key], w=["wop%d" % c])
                stage_load([(lambda t: t[:, 0:1024], dr["w_out"][c * 128:(c + 1) * 128, :])], cons)
            for hl in range(4):
                def cons(t, key, hl=hl):
                    T.op("act", lambda e: e.activation(out=woa[:, hl, :], in_=t[:, 0:1024], func=AF.Copy), r=[key], w=["woa%d" % hl])
                stage_load([(lambda t: t[0:64, 0:1024], dr["w_out"][512 + hl * 64:512 + (hl + 1) * 64, :]),
                            (lambda t: t[64:128, 0:1024], dr["w_out"][512 + (4 + hl) * 64:512 + (5 + hl) * 64, :])], cons)

            def cons(t, key):
                T.op("act", lambda e: e.activation(out=wpl[:].rearrange("p g d -> p (g d)"), in_=t[:, 0:512], func=AF.Copy),
                     r=[key], w=["wpl"])
            stage_load([(lambda t: t[:, 0:512].rearrange("p (g d) -> p g d", g=4), dr["w_pool"].rearrange("g c d -> c g d"))], cons)

            def P(fn, r=(), w=()):
                T.op("pool", fn, r=r, w=w)

            def V(fn, r=(), w=()):
                T.op("dve", fn, r=r, w=w)

            def A(fn, r=(), w=()):
                T.op("act", fn, r=r, w=w)

            P(lambda e: e.memset(eps_t[:], EPS), w=["eps_t"])
            P(lambda e: e.memset(identf[:], 1.0), w=["identf"])
            P(lambda e: e.affine_select(out=identf[:], in_=identf[:], pattern=[[-1, 128]], compare_op=ALU.is_equal,
                                        fill=0.0, base=0, channel_multiplier=1), r=["identf"], w=["identf"])
            V(lambda e: e.tensor_copy(out=ident[:], in_=identf[:]), r=["identf"], w=["ident"])
            P(lambda e: e.memset(mask_d[:], 1.0), w=["mask_d"])
            P(lambda e: e.affine_select(out=mask_d[:], in_=mask_d[:], pattern=[[1, 128]], compare_op=ALU.is_ge,
                                        fill=0.0, base=0, channel_multiplier=-1), r=["mask_d"], w=["mask_d"])
            P(lambda e: e.memset(mask_p[:], 1.0), w=["mask_p"])
            P(lambda e: e.affine_select(out=mask_p[:], in_=mask_p[:], pattern=[[-1, 128]], compare_op=ALU.is_gt,
                                        fill=0.0, base=0, channel_multiplier=1), r=["mask_p"], w=["mask_p"])
            for mk_, ng_, nm_ in ((mask_d, negm_d, "negm_d"), (mask_p, negm_p, "negm_p")):
                V(lambda e, mk_=mk_, ng_=ng_: e.tensor_scalar(out=ng_[:], in0=mk_[:].unsqueeze(1).to_broadcast([128, 4, 128]),
                                                             scalar1=-1.0, scalar2=30000.0, op0=ALU.add, op1=ALU.mult),
                  r=["mask_d", "mask_p"], w=[nm_])
            P(lambda e: e.memset(ones64[:], 1.0), w=["ones64"])
            P(lambda e: e.memset(ones128[:], 1.0), w=["ones128"])
            P(lambda e: e.memset(zt[:], 0.0), w=["zt"])
            P(lambda e: e.memset(tri[:], 1.0), w=["tri"])
            P(lambda e: e.affine_select(out=tri[:], in_=tri[:], pattern=[[1, 128]], compare_op=ALU.is_gt,
                                        fill=0.0, base=0, channel_multiplier=-1), r=["tri"], w=["tri"])
            P(lambda e: e.memset(onesm[:], 0.0), w=["onesm"])
            P(lambda e: e.memset(onesm[0:16, :], 1.0), r=["onesm"], w=["onesm"])
            P(lambda e: e.memset(onesm[32:33, :], 1.0), r=["onesm"], w=["onesm"])
            P(lambda e: e.memset(sel[:], 1.0), w=["sel"])
            P(lambda e: e.affine_select(out=sel[:], in_=sel[:], pattern=[[1, 16], [0, 128]], compare_op=ALU.is_equal,
                                        fill=0.0, base=0, channel_multiplier=-1), r=["sel"], w=["sel"])
            for g, w_ in enumerate(POOLW):
                iw = 1.0 / w_
                k = "Pf%d" % g
                P(lambda e, g=g, iw=iw: e.memset(Pf[:, g, :], iw), w=[k])
                P(lambda e, g=g: e.affine_select(out=Pf[:, g, :], in_=Pf[:, g, :], pattern=[[1, 128]], compare_op=ALU.is_ge,
                                                 fill=0.0, base=0, channel_multiplier=-1), r=[k], w=[k])
                P(lambda e, g=g, w_=w_: e.affine_select(out=Pf[:, g, :], in_=Pf[:, g, :], pattern=[[-1, 128]], compare_op=ALU.is_ge,
                                                        fill=0.0, base=w_ - 1, channel_multiplier=1), r=[k], w=[k])
                V(lambda e, g=g: e.tensor_tensor(out=Pd[:, g, :], in0=Pf[:, g, :], in1=identf[:], op=ALU.subtract),
                  r=[k, "identf"], w=["Pd"])
                k2 = "Pf%d" % (4 + g)
                P(lambda e, g=g, iw=iw: e.memset(Pf[:, 4 + g, :], iw), w=[k2])
                P(lambda e, g=g, w_=w_: e.affine_select(out=Pf[:, 4 + g, :], in_=Pf[:, 4 + g, :], pattern=[[-1, 128]],
                                                        compare_op=ALU.is_ge, fill=0.0, base=w_ - 129, channel_multiplier=1),
                  r=[k2], w=[k2])
                V(lambda e, g=g: e.tensor_copy(out=Pp[:, g, :], in_=Pf[:, 4 + g, :]), r=[k2], w=["Pp"])
                k3 = "Pf%d" % (8 + g)
                P(lambda e, g=g, iw=iw: e.memset(Pf[0:16, 8 + g, :], iw), w=[k3])
                P(lambda e, g=g, w_=w_: e.affine_select(out=Pf[0:16, 8 + g, :], in_=Pf[0:16, 8 + g, :], pattern=[[-1, 128]],
                                                        compare_op=ALU.is_ge, fill=0.0, base=w_ - 17, channel_multiplier=1),
                  r=[k3], w=[k3])
                V(lambda e, g=g: e.tensor_copy(out=Pm[:, g, :], in_=Pf[0:16, 8 + g, :]), r=[k3], w=["Pm"])
            P(lambda e: e.iota(posi[:, 1:17], pattern=[[128, 16]], base=16, channel_multiplier=1), w=["posi_a"])
            P(lambda e: e.iota(posi[:, 0:1], pattern=[[0, 1]], base=0, channel_multiplier=1), w=["posi_b"])
            V(lambda e: e.tensor_copy(out=posf[:], in_=posi[:]), r=["posi_a", "posi_b"], w=["posf"])
            for i in range(8):
                V(lambda e, i=i: e.tensor_scalar(out=ang[:, :, i], in0=posf[:], scalar1=INV_FREQ[i] / (2 * math.pi),
                                                 scalar2=None, op0=ALU.mult), r=["posf"], w=["ang"])

            def trig(dst, shift, nm):
                V(lambda e: e.tensor_scalar(out=tA[:], in0=ang[:], scalar1=shift, scalar2=None, op0=ALU.add),
                  r=["ang"], w=["tA"])
                V(lambda e: e.tensor_copy(out=angi[:], in_=tA[:]), r=["tA"], w=["angi"])
                V(lambda e: e.tensor_copy(out=angf[:], in_=angi[:]), r=["angi"], w=["angf"])
                V(lambda e: e.tensor_tensor(out=tA[:], in0=tA[:], in1=angf[:], op=ALU.subtract), r=["tA", "angf"], w=["tA"])
                V(lambda e: e.tensor_scalar(out=tB[:], in0=tA[:], scalar1=0.5, scalar2=None, op0=ALU.is_gt), r=["tA"], w=["tB"])
                V(lambda e: e.tensor_tensor(out=tA[:], in0=tA[:], in1=tB[:], op=ALU.subtract), r=["tA", "tB"], w=["tA"])
                V(lambda e: e.tensor_scalar(out=tB[:], in0=tA[:], scalar1=-0.5, scalar2=None, op0=ALU.is_lt), r=["tA"], w=["tB"])
                V(lambda e: e.tensor_tensor(out=tA[:], in0=tA[:], in1=tB[:], op=ALU.add), r=["tA", "tB"], w=["tA"])
                A(lambda e: e.activation(out=dst[:], in_=tA[:], func=AF.Sin, scale=2 * math.pi), r=["tA"], w=[nm])

            trig(sinT, 0.0, "sinT")
            trig(cosT, 0.25, "cosT")
            V(lambda e: e.tensor_copy(out=qkgain[:, 0:8, :], in_=qg_b[:].unsqueeze(1).to_broadcast([128, 8, 64])),
              r=["qg_b"], w=["qkgain_a"])
            V(lambda e: e.tensor_copy(out=qkgain[:, 8:10, :], in_=kg_b[:].unsqueeze(1).to_broadcast([128, 2, 64])),
              r=["kg_b"], w=["qkgain_b"])
            V(lambda e: e.reduce_max(out=mq[:], in_=qg_b[:], axis=AX.X, apply_absolute_value=True), r=["qg_b"], w=["mq"])
            V(lambda e: e.reduce_max(out=mk[:], in_=kg_b[:], axis=AX.X, apply_absolute_value=True), r=["kg_b"], w=["mk"])
            V(lambda e: e.tensor_tensor(out=negC[:], in0=mq[:], in1=mk[:], op=ALU.mult), r=["mq", "mk"], w=["negC"])
            V(lambda e: e.tensor_scalar(out=negC[:], in0=negC[:], scalar1=-8.0, scalar2=None, op0=ALU.mult),
              r=["negC"], w=["negC"])
            A(lambda e: e.activation(out=es[32:33, :], in_=sk[32:33, :], func=AF.Exp, bias=negC[32:33, :]),
              r=["sk", "negC"], w=["es"])
            for g in range(2):
                for s in range(2):
                    k = "PTm%d_%d" % (g, s)
                    P(lambda e, g=g, s=s: e.memset(PTm[g][s][:], 0.0), w=[k])
                    V(lambda e, g=g, s=s: e.tensor_copy(
                        out=PTm[g][s][32:33, :].rearrange("p (h q) -> p h q", h=4),
                        in_=es[32:33, 4 * g:4 * g + 4].unsqueeze(2).to_broadcast([1, 4, 128])), r=["es", k], w=[k])
            for c in range(8):
                V(lambda e, c=c: e.tensor_copy(out=wr[:, c, :], in_=wrs[:, c, :]), r=["wrs_a", "wrs_b"], w=["wr"])
            T.flush()

        def phase1(rnd, do_meta):
            PFX = "p1r%d" % rnd
            TPS = SEQ // 128
            with contextlib.ExitStack() as s1:
                def sb1(name, shape, dt=F32):
                    return s1.enter_context(nc.sbuf_tensor("%s_%s" % (PFX, name), list(shape), dt))

                def ps1(name, shape, dt=F32):
                    return s1.enter_context(nc.psum_tensor("%s_%s" % (PFX, name), list(shape), dt))

                xt = [sb1("xt%d" % i, [128, 1024]) for i in range(5)]
                junk = sb1("junk", [128, 1024], BF16)
                junkC = sb1("junkC", [128, 1024], BF16)
                xb = [sb1("xb%d" % i, [128, 1024], BF16) for i in range(2)]
                xT = [sb1("xT%d" % i, [128, 8, 128], BF16) for i in range(2)]
                qk = [sb1("qk%d" % i, [128, 640]) for i in range(2)]
                sq = sb1("sq", [128, 640])
                qn = [sb1("qn%d" % i, [128, 10, 64]) for i in range(2)]
                qf = [sb1("qf%d" % i, [128, 10, 64], BF16) for i in range(2)]
                rt = [sb1("rt%d" % i, [128, 10, 8]) for i in range(4)]
                st_ = [sb1("st%d" % i, [128, 16]) for i in range(2)]
                hs = [sb1("hs%d" % i, [128, 10]) for i in range(2)]
                hr = [sb1("hr%d" % i, [128, 10]) for i in range(2)]
                QT = [sb1("QT%d" % i, [128, 4, 128], BF16) for i in range(2)]
                PTd = [[sb1("PTd%d_%d" % (g, i), [128, 512], BF16) for i in range(2)] for g in range(2)]
                PTp = [[sb1("PTp%d_%d" % (g, i), [128, 512], BF16) for i in range(2)] for g in range(2)]
                rD = sb1("rD", [128, 512])
                yT = [sb1("yT%d" % i, [128, 512], BF16) for i in range(2)]
                mixT = [sb1("mixT%d" % i, [128, 4, 128], BF16) for i in range(2)]
                ypT = [sb1("ypT%d" % i, [128, 4, 128], BF16) for i in range(2)]
                mb = [sb1("mb%d" % i, [128, 1024], BF16) for i in range(2)]
                st2 = [sb1("st2_%d" % i, [128, 4]) for i in range(2)]
                rs = [sb1("rs%d" % i, [128, 96]) for i in range(2)]
                vm_tmp = sb1("vm_tmp", [128, 128], BF16)
                h1t = [sb1("h1t%d" % i, [128, 1024]) for i in range(2)]
                mTt = [sb1("mTt%d" % i, [128, 8, 128], BF16) for i in range(2)]
                cstg = [sb1("cstg%d" % i, [128, 1024]) for i in range(3)]
                wtile = [sb1("wtile%d" % i, [128, WROW], BF16) for i in range(2)]
                cidx = [0]

                tp = ps1("tp", [128, 1024], BF16)
                b1 = ps1("b1", [128, 512])
                b2 = ps1("b2", [128, 512])
                b3 = ps1("b3", [128, 512])
                b4 = ps1("b4", [128, 512])
                b5 = ps1("b5", [128, 512])
                b6 = ps1("b6", [128, 512])
                b7 = ps1("b7", [128, 512])

                def Pq(fn, r=(), w=()):
                    T.op("pool", fn, r=r, w=w)

                def Vq(fn, r=(), w=()):
                    T.op("dve", fn, r=r, w=w)

                def Aq(fn, r=(), w=()):
                    T.op("act", fn, r=r, w=w)

                def Eq(fn, r=(), w=()):
                    T.op("pe", fn, r=r, w=w)

                def rstd_chain(src, n_inv, dst, lnbuf, kin, kln, kout):
                    Aq(lambda e: e.activation(out=lnbuf, in_=src, func=AF.Ln, scale=n_inv, bias=eps_t[:]),
                       r=[kin, "eps_t"], w=[kln])
                    Aq(lambda e: e.activation(out=dst, in_=lnbuf, func=AF.Exp, scale=-0.5), r=[kln], w=[kout])

                def stageA(a, src_rows, c, meta=False, gt=None, part="both"):
                    s2 = a % 2
                    s3 = a % 5
                    kx = "xt%d" % s3
                    ss = st_[s2][:, 0:1]
                    lnv = st_[s2][:, 1:2]
                    rstd = st_[s2][:, 2:3]
                    kss, kln, krs = "ss%d" % s2, "lnv%d" % s2, "rstd%d" % s2
                    kxb = "xb%d" % s2
                    kxT = "xT%d" % s2
                    if part in ("both", "early"):
                        if meta:
                            Pq(lambda e: e.memset(xt[s3][:], 0.0), w=[kx])
                            T.op("sp", lambda e: e.dma_start(out=xt[s3][0:16, :], in_=dr["meta"]), r=[kx], w=[kx], dma="D_" + kx)
                        else:
                            T.seg(0, 3)
                        Aq(lambda e: e.activation(out=junk[:], in_=xt[s3][:], func=AF.Square, accum_out=ss), r=[kx], w=["junk", kss])
                        rstd_chain(ss, 1.0 / D, rstd, lnv, kss, kln, krs)
                        Vq(lambda e: e.tensor_copy(out=xb[s2][:], in_=xt[s3][:]), r=[kx], w=[kxb])

                        def t1(e):
                            for k in range(8):
                                ins = e.transpose(out=tp[:, k * 128:(k + 1) * 128], in_=xb[s2][:, k * 128:(k + 1) * 128], identity=ident[:])
                            return ins
                        Eq(t1, r=[kxb, "ident"], w=["tp"])
                        Vq(lambda e: e.tensor_copy(out=xT[s2][:].rearrange("p k t -> p (k t)"), in_=tp[:]), r=["tp"], w=[kxT])
                        if part == "early":
                            return
                    if not meta:
                        T.seg(2, 0)

                    def mm(bank, c0, c1):
                        def f(e):
                            for k in range(8):
                                ins = e.matmul(bank[:, 0:c1 - c0], lhsT=xT[s2][:, k, :], rhs=win[:, k, c0:c1],
                                               start=(k == 0), stop=(k == 7))
                            return ins
                        return f
                    Eq(mm(b1, 0, 512), r=[kxT, "win"], w=["b1_0", "b1_1"])
                    Eq(mm(b2, 512, 1024), r=[kxT, "win"], w=["b2_0", "b2_1"])
                    Eq(mm(b3, 1024, 1280), r=[kxT, "win"], w=["b3"])
                    if meta:
                        ubuf, kub = um, "um"
                        vbuf, kvb = vm_tmp, "vm_tmp"
                        kbuf, kkb = KTm, "KTm"
                    else:
                        ubuf, kub = uring[gt % 3], "ur%d" % (gt % 3)
                        vbuf, kvb = vring[gt % 3], "vr%d" % (gt % 3)
                        kbuf, kkb = kring[gt % 3], "kr%d" % (gt % 3)
                    Aq(lambda e: e.activation(out=ubuf[:], in_=b1[:], func=AF.Copy, scale=rstd), r=["b1_0", "b1_1", krs], w=[kub])
                    kqk = "qk%d" % s2
                    Aq(lambda e: e.activation(out=qk[s2][:, 0:512], in_=b2[:], func=AF.Copy, scale=rstd), r=["b2_0", "b2_1", krs], w=[kqk + "a"])
                    Aq(lambda e: e.activation(out=qk[s2][:, 512:640], in_=b3[:, 0:128], func=AF.Copy, scale=rstd),
                       r=["b3", krs], w=[kqk + "b"])
                    Aq(lambda e: e.activation(out=vbuf[:], in_=b3[:, 128:256], func=AF.Copy, scale=rstd), r=["b3", krs], w=[kvb])
                    if not meta:
                        T.seg(3, 0)
                    Pq(lambda e: e.tensor_tensor(out=sq[:], in0=qk[s2][:], in1=qk[s2][:], op=ALU.mult),
                       r=[kqk + "a", kqk + "b"], w=["sq"])
                    khs, khl, khr = "hs%d" % s2, "hl%d" % s2, "hr%d" % s2
                    Vq(lambda e: e.tensor_reduce(out=hs[s2][:], in_=sq[:].rearrange("p (h d) -> p h d", h=10), axis=AX.X, op=ALU.add),
                       r=["sq"], w=[khs])
                    rstd_chain(hs[s2][:], 1.0 / 64, hr[s2][:], hs[s2][:], khs, khs, khr)
                    kqn = "qn%d" % s2
                    Vq(lambda e: e.tensor_tensor(out=qn[s2][:], in0=qk[s2][:].rearrange("p (h d) -> p h d", h=10),
                                                 in1=hr[s2][:].unsqueeze(2).to_broadcast([128, 10, 64]), op=ALU.mult),
                       r=[kqk + "a", kqk + "b", khr], w=[kqn])
                    Pq(lambda e: e.tensor_tensor(out=qn[s2][:], in0=qn[s2][:], in1=qkgain[:], op=ALU.mult),
                       r=[kqn, "qkgain_a", "qkgain_b"], w=[kqn])
                    if not meta:
                        T.seg(4, 0)
                    cs = cosT[:, c, :].unsqueeze(1).to_broadcast([128, 10, 8])
                    sn = sinT[:, c, :].unsqueeze(1).to_broadcast([128, 10, 8])
                    x1 = qn[s2][:, :, 0:8]
                    x2 = qn[s2][:, :, 8:16]
                    kqf = "qf%d" % s2
                    Pq(lambda e: e.tensor_tensor(out=rt[0][:], in0=x1, in1=cs, op=ALU.mult), r=[kqn, "cosT"], w=["rt0"])
                    Pq(lambda e: e.tensor_tensor(out=rt[1][:], in0=x2, in1=sn, op=ALU.mult), r=[kqn, "sinT"], w=["rt1"])
                    Pq(lambda e: e.tensor_tensor(out=qf[s2][:, :, 0:8], in0=rt[0][:], in1=rt[1][:], op=ALU.subtract),
                       r=["rt0", "rt1"], w=[kqf + "a"])
                    Pq(lambda e: e.tensor_tensor(out=rt[2][:], in0=x2, in1=cs, op=ALU.mult), r=[kqn, "cosT"], w=["rt2"])
                    Pq(lambda e: e.tensor_tensor(out=rt[3][:], in0=x1, in1=sn, op=ALU.mult), r=[kqn, "sinT"], w=["rt3"])
                    Pq(lambda e: e.tensor_tensor(out=qf[s2][:, :, 8:16], in0=rt[2][:], in1=rt[3][:], op=ALU.add),
                       r=["rt2", "rt3"], w=[kqf + "b"])
                    Pq(lambda e: e.tensor_copy(out=qf[s2][:, :, 16:64], in_=qn[s2][:, :, 16:64]), r=[kqn], w=[kqf + "c"])
                    qff = qf[s2][:].rearrange("p h d -> p (h d)")
                    if not meta:
                        T.seg(5, 0)

                    def t2(e):
                        for b in range(5):
                            ins = e.transpose(out=tp[:, b * 128:(b + 1) * 128], in_=qff[:, b * 128:(b + 1) * 128], identity=ident[:])
                        return ins
                    Eq(t2, r=[kqf + "a", kqf + "b", kqf + "c", "ident"], w=["tp"])
                    if not meta:
                        Vq(lambda e: e.tensor_copy(out=QT[s2][:].rearrange("p b t -> p (b t)"), in_=tp[:, 0:512]),
                           r=["tp"], w=["QT%d" % s2])
                    Vq(lambda e: e.tensor_copy(out=kbuf[:], in_=tp[:, 512:640]), r=["tp"], w=[kkb])
                    if meta:
                        for g in range(2):
                            Pq(lambda e, g=g: e.memset(Vm[g][:], 0.0), w=["Vm%d" % g])
                            Pq(lambda e, g=g: e.tensor_copy(out=Vm[g][0:16, :], in_=vm_tmp[0:16, g * 64:(g + 1) * 64]),
                               r=["vm_tmp", "Vm%d" % g], w=["Vm%d" % g])

                def stageB(a, gt, first):
                    s2 = a % 2
                    cur, prv = gt % 3, (gt - 1) % 3
                    kuc, kup = "ur%d" % cur, "ur%d" % prv
                    T.seg(0, 1)

                    def pm(e):
                        for gp in range(4):
                            e.matmul(b7[:, gp * 128:(gp + 1) * 128], lhsT=uring[cur][:, gp * 128:(gp + 1) * 128], rhs=Pd[:, gp, :],
                                     start=True, stop=False)
                            if first:
                                ins = e.matmul(b7[:, gp * 128:(gp + 1) * 128], lhsT=um[0:16, gp * 128:(gp + 1) * 128],
                                               rhs=Pm[0:16, gp, :], start=False, stop=True)
                            else:
                                ins = e.matmul(b7[:, gp * 128:(gp + 1) * 128], lhsT=uring[prv][64:128, gp * 128:(gp + 1) * 128],
                                               rhs=Pp[64:128, gp, :], start=False, stop=True)
                        return ins
                    Eq(pm, r=[kuc, "um" if first else kup, "Pd", "Pp", "Pm"], w=["b7"])
                    kmx = "mixT%d" % s2
                    Aq(lambda e: e.activation(out=mixT[s2][:].rearrange("p g t -> p (g t)"), in_=b7[:], func=AF.Copy),
                       r=["b7"], w=[kmx])

                    def pl(e):
                        for gp in range(4):
                            ins = e.matmul(b7[:, gp * 128:(gp + 1) * 128], lhsT=wpl[:, gp, :], rhs=mixT[s2][:, gp, :],
                                           start=True, stop=True)
                        return ins
                    Eq(pl, r=[kmx, "wpl"], w=["b7"])
                    kyp = "ypT%d" % s2
                    Vq(lambda e: e.tensor_tensor(out=ypT[s2][:], in0=b7[:].rearrange("p (g t) -> p g t", g=4),
                                                 in1=ps_t[:].unsqueeze(2).to_broadcast([128, 4, 128]), op=ALU.mult),
                       r=["b7", "ps_t"], w=[kyp])
                    kq = "QT%d" % s2
                    for g in range(2):
                        pr = slice(g * 64, (g + 1) * 64)
                        qrhs = QT[s2][pr, :, :].rearrange("p b t -> p (b t)")
                        T.seg(1 if g == 0 else 4, 1)
                        def sd(e, pr=pr, qrhs=qrhs):
                            e.matmul(b4[:], lhsT=kring[cur][pr, :], rhs=qrhs, start=True, stop=False)
                            return e.matmul(b4[:], lhsT=ident[:], rhs=negm_d[:].rearrange("p h q -> p (h q)"), start=False, stop=True)
                        Eq(sd, r=["kr%d" % cur, kq, "ident", "negm_d"], w=["b4"])
                        if not first:
                            def sp_(e, pr=pr, qrhs=qrhs):
                                e.matmul(b5[:], lhsT=kring[prv][pr, :], rhs=qrhs, start=True, stop=False)
                                return e.matmul(b5[:], lhsT=ident[:], rhs=negm_p[:].rearrange("p h q -> p (h q)"), start=False, stop=True)
                            Eq(sp_, r=["kr%d" % prv, kq, "ident", "negm_p"], w=["b5"])
                        Eq(lambda e, pr=pr, qrhs=qrhs: e.matmul(b6[0:16, :], lhsT=KTm[pr, 0:16], rhs=qrhs, start=True, stop=True),
                           r=["KTm", kq], w=["b6"])
                        kd, kp, km = "PTd%d_%d" % (g, s2), "PTp%d_%d" % (g, s2), "PTm%d_%d" % (g, s2)
                        Aq(lambda e, g=g: e.activation(out=PTd[g][s2][:], in_=b4[:], func=AF.Exp, scale=0.125, bias=negC[:]),
                           r=["b4", "negC"], w=[kd])
                        if not first:
                            Aq(lambda e, g=g: e.activation(out=PTp[g][s2][:], in_=b5[:], func=AF.Exp, scale=0.125, bias=negC[:]),
                               r=["b5", "negC"], w=[kp])
                        Aq(lambda e, g=g: e.activation(out=PTm[g][s2][0:16, :], in_=b6[0:16, :], func=AF.Exp, scale=0.125,
                                                       bias=negC[0:16, :]), r=["b6", "negC"], w=[km])
                        T.seg(3 if g == 0 else 5, 1)

                        def pv(e, g=g, pr=pr):
                            blocks = [(vring[cur][:, pr], ones64[:, :], PTd[g][s2][:, :])]
                            if not first:
                                blocks.append((vring[prv][:, pr], ones64[:, :], PTp[g][s2][:, :]))
                            blocks.append((Vm[g][0:33, :], onesm[0:33, :], PTm[g][s2][0:33, :]))
                            n = len(blocks)
                            for i, (vl, ol, rh) in enumerate(blocks):
                                e.matmul(b1[pr, :], lhsT=vl, rhs=rh, start=(i == 0), stop=(i == n - 1))
                            for i, (vl, ol, rh) in enumerate(blocks):
                                ins = e.matmul(b2[pr, :], lhsT=ol, rhs=rh, start=(i == 0), stop=(i == n - 1))
                            return ins
                        rr = ["vr%d" % cur, kd, km, "Vm%d" % g, "ones64", "onesm"]
                        if not first:
                            rr += ["vr%d" % prv, kp]
                        Eq(pv, r=rr, w=["b1_%d" % g, "b2_%d" % g])
                        krd = "rD%d" % (g)
                        Aq(lambda e, pr=pr: e.activation(out=rD[pr, :], in_=b2[pr, :], func=AF.Ln), r=["b2_%d" % g], w=[krd])
                        Aq(lambda e, pr=pr: e.activation(out=rD[pr, :], in_=rD[pr, :], func=AF.Exp, scale=-1.0), r=[krd], w=[krd])
                        Vq(lambda e, pr=pr: e.tensor_tensor(out=yT[s2][pr, :], in0=b1[pr, :], in1=rD[pr, :], op=ALU.mult),
                           r=["b1_%d" % g, krd], w=["yT%d_%d" % (g, s2)])

                def stageC(a, lt):
                    s2 = a % 2
                    s3 = a % 5
                    kx = "xt%d" % s3
                    kyp = "ypT%d" % s2
                    gtt = rnd * NT + lt
                    T.seg(2, 2)

                    def wo(e):
                        for nh, bank in enumerate((b4, b5)):
                            cols = slice(nh * 512, (nh + 1) * 512)
                            for gp in range(4):
                                e.matmul(bank[:], lhsT=ypT[s2][:, gp, :], rhs=wop[:, gp, cols], start=(gp == 0), stop=False)
                            for hl in range(4):
                                ins = e.matmul(bank[:], lhsT=yT[s2][:, hl * 128:(hl + 1) * 128], rhs=woa[:, hl, cols],
                                               start=False, stop=(hl == 3))
                        return ins
                    Eq(wo, r=[kyp, "yT0_%d" % s2, "yT1_%d" % s2, "wop", "woa"], w=["b4", "b5"])
                    ky = "h1t%d" % s2
                    Vq(lambda e: e.tensor_tensor(out=h1t[s2][:, 0:512], in0=b4[:], in1=xt[s3][:, 0:512], op=ALU.add),
                       r=["b4", kx], w=[ky + "a"])
                    Vq(lambda e: e.tensor_tensor(out=h1t[s2][:, 512:1024], in0=b5[:], in1=xt[s3][:, 512:1024], op=ALU.add),
                       r=["b5", kx], w=[ky + "b"])
                    r0 = gtt * 128
                    T.op("sp", lambda e: e.dma_start(out=out[r0:r0 + 128, :], in_=h1t[s2][:]), r=[ky + "a", ky + "b"],
                         dma="S_h1t%d" % s2)
                    ss = st2[s2][:, 0:1]
                    lnv = st2[s2][:, 1:2]
                    rstd = st2[s2][:, 2:3]
                    kss, kln, krs = "ss2_%d" % s2, "lnv2_%d" % s2, "rstd2_%d" % s2
                    T.seg(3, 2)
                    Aq(lambda e: e.activation(out=junkC[:], in_=h1t[s2][:], func=AF.Square, accum_out=ss),
                       r=[ky + "a", ky + "b"], w=["junkC", kss])
                    rstd_chain(ss, 1.0 / D, rstd, lnv, kss, kln, krs)
                    kmb = "mb%d" % s2
                    Vq(lambda e: e.scalar_tensor_tensor(out=mb[s2][:], in0=h1t[s2][:], scalar=rstd, in1=fgb[:],
                                                        op0=ALU.mult, op1=ALU.mult),
                       r=[ky + "a", ky + "b", krs, "fgb"], w=[kmb])
                    T.op("sp", lambda e: e.dma_start(out=mscr[r0:r0 + 128, :], in_=mb[s2][:]), r=[kmb], dma="S_mb%d" % s2)

                    T.seg(4, 2)

                    def t3(e):
                        for k in range(8):
                            ins = e.transpose(out=tp[:, k * 128:(k + 1) * 128], in_=mb[s2][:, k * 128:(k + 1) * 128], identity=ident[:])
                        return ins
                    Eq(t3, r=[kmb, "ident"], w=["tp"])
                    kmt = "mTt%d" % s2
                    Vq(lambda e: e.tensor_copy(out=mTt[s2][:].rearrange("p k t -> p (k t)"), in_=tp[:]), r=["tp"], w=[kmt])

                    T.seg(5, 2)

                    def rt_(e):
                        for k in range(8):
                            ins = e.matmul(b3[:, 256:276], lhsT=mTt[s2][:, k, :], rhs=wr[:, k, :],
                                           start=(k == 0), stop=(k == 7))
                        return ins
                    Eq(rt_, r=[kmt, "wr"], w=["b3"])
                    R = rs[s2]
                    kr = "rs%d" % s2
                    lgs = R[:, 0:20]
                    gmax, ngmax, gsum, gprob = R[:, 20:21], R[:, 21:22], R[:, 22:23], R[:, 23:24]
                    goh = R[:, 24:28]
                    gexp = R[:, 28:32]
                    tmp = R[:, 32:48]
                    ig = R[:, 48:52]
                    m1, m2, d21, aa = R[:, 52:53], R[:, 53:54], R[:, 54:55], R[:, 55:56]
                    oh1, msk, oh2 = R[:, 56:60], R[:, 60:64], R[:, 64:68]
                    den = R[:, 68:69]
                    w1 = w12[:, gtt, 0:1]
                    w2 = w12[:, gtt, 1:2]
                    kw = "w12_%d" % gtt

                    def RV(fn):
                        Vq(fn, r=[kr], w=[kr])
                    Vq(lambda e: e.tensor_copy(out=lgs, in_=b3[:, 256:276]), r=["b3"], w=[kr])
                    RV(lambda e: e.reduce_max(out=gmax, in_=lgs[:, 0:4], axis=AX.X))
                    RV(lambda e: e.tensor_scalar(out=goh, in0=lgs[:, 0:4], scalar1=gmax, scalar2=None, op0=ALU.is_ge))
                    RV(lambda e: e.tensor_scalar(out=ngmax, in0=gmax, scalar1=-1.0, scalar2=None, op0=ALU.mult))
                    Aq(lambda e: e.activation(out=gexp, in_=lgs[:, 0:4], func=AF.Exp, bias=ngmax, accum_out=gsum), r=[kr], w=[kr])
                    RV(lambda e: e.reciprocal(out=gprob, in_=gsum))
                    RV(lambda e: e.tensor_tensor(out=tmp.rearrange("p (g e) -> p g e", g=4),
                                                 in0=lgs[:, 4:20].rearrange("p (g e) -> p g e", g=4),
                                                 in1=goh.unsqueeze(2).to_broadcast([128, 4, 4]), op=ALU.mult))
                    RV(lambda e: e.tensor_reduce(out=ig, in_=tmp.rearrange("p (g e) -> p e g", g=4), axis=AX.X, op=ALU.add))
                    RV(lambda e: e.reduce_max(out=m1, in_=ig, axis=AX.X))
                    RV(lambda e: e.tensor_scalar(out=oh1, in0=ig, scalar1=m1, scalar2=None, op0=ALU.is_ge))
                    RV(lambda e: e.scalar_tensor_tensor(out=msk, in0=oh1, scalar=-1e30, in1=ig, op0=ALU.mult, op1=ALU.add))
                    RV(lambda e: e.reduce_max(out=m2, in_=msk, axis=AX.X))
                    RV(lambda e: e.tensor_scalar(out=oh2, in0=msk, scalar1=m2, scalar2=None, op0=ALU.is_ge))
                    RV(lambda e: e.tensor_tensor(out=d21, in0=m2, in1=m1, op=ALU.subtract))
                    Aq(lambda e: e.activation(out=aa, in_=d21, func=AF.Exp), r=[kr], w=[kr])
                    RV(lambda e: e.tensor_scalar(out=den, in0=aa, scalar1=1.0, scalar2=None, op0=ALU.add))
                    RV(lambda e: e.reciprocal(out=den, in_=den))
                    Vq(lambda e: e.tensor_tensor(out=w1, in0=den, in1=gprob, op=ALU.mult), r=[kr], w=[kw + "a"])
                    Vq(lambda e: e.tensor_tensor(out=w2, in0=w1, in1=aa, op=ALU.mult), r=[kr, kw + "a"], w=[kw + "b"])
                    ke = "E_%d" % gtt
                    Vq(lambda e: e.tensor_tensor(out=E1all[:, gtt, :].rearrange("p (g e) -> p g e", g=4),
                                                 in0=goh.unsqueeze(2).to_broadcast([128, 4, 4]),
                                                 in1=oh1.unsqueeze(1).to_broadcast([128, 4, 4]), op=ALU.mult), r=[kr], w=[ke + "1"])
                    Vq(lambda e: e.tensor_tensor(out=E2all[:, gtt, :].rearrange("p (g e) -> p g e", g=4),
                                                 in0=goh.unsqueeze(2).to_broadcast([128, 4, 4]),
                                                 in1=oh2.unsqueeze(1).to_broadcast([128, 4, 4]), op=ALU.mult), r=[kr], w=[ke + "2"])
                    Vq(lambda e: e.tensor_tensor(out=Aall[:, gtt, :], in0=E1all[:, gtt, :], in1=E2all[:, gtt, :], op=ALU.add),
                       r=[ke + "1", ke + "2"], w=["A_%d" % gtt])

                PIECES = [("g", 0), ("u", 0), ("g", 1), ("u", 1), ("d", 0), ("d", 1)]

                def conv_load(k):
                    e_, pi = divmod(k, 6)
                    kind, hh = PIECES[pi]
                    s_ = k % 3
                    key = "cstg%d" % s_
                    if kind in ("g", "u"):
                        src = dr["w_gate" if kind == "g" else "w_up"][e_, hh * 512:(hh + 1) * 512, :].rearrange(
                            "(k p) f -> p k f", p=128)
                        dst = cstg[s_][:].rearrange("p (k f) -> p k f", k=4)
                    else:
                        src = dr["w_down"][e_, hh * 128:(hh + 1) * 128, :]
                        dst = cstg[s_][:]
                    T.op("act", lambda e: e.dma_start(out=dst, in_=src), w=[key], dma="D_" + key)

                def conv_cast(k):
                    e_, pi = divmod(k, 6)
                    kind, hh = PIECES[pi]
                    s_ = k % 3
                    key = "cstg%d" % s_
                    par = e_ % 2
                    kwt = "wtile%d" % par
                    if kind in ("g", "u"):
                        off = 0 if kind == "g" else 256
                        wv = wtile[par][:, 0:4096].rearrange("p (k f) -> p k f", k=8)
                        T.op("act", lambda e: e.activation(out=wv[:, hh * 4:(hh + 1) * 4, off:off + 256],
                                                           in_=cstg[s_][:].rearrange("p (k f) -> p k f", k=4), func=AF.Copy),
                             r=[key], w=[kwt + kind + str(hh)])
                    else:
                        T.op("act", lambda e: e.activation(out=wtile[par][:, 4096 + hh * 1024:4096 + (hh + 1) * 1024],
                                                           in_=cstg[s_][:], func=AF.Copy),
                             r=[key], w=[kwt + kind + str(hh)])
                    if pi == 5:
                        T.op("act", lambda e: e.dma_start(out=wscr[e_ * 128:(e_ + 1) * 128, :], in_=wtile[par][:]),
                             r=[kwt + k_ + str(h) for k_ in "gud" for h in range(2)], dma="S_" + kwt)

                cnt = rnd * NT + (1 if True else 0)
                if do_meta:
                    stageA(0, None, 0, meta=True)
                base_gt = rnd * NT

                def xload(lt):
                    s5 = (lt + 1) % 5
                    rows = dr["x"][lt * 128:(lt + 1) * 128, :]
                    T.op("sp", lambda e: e.dma_start(out=xt[s5][:], in_=rows), w=["xt%d" % s5], dma="D_xt%d" % s5)

                xload(0)
                xload(1)
                conv_load(0)
                for step in range(NT + 2):
                    if step + 2 < NT:
                        T.seg(-1, 0)
                        xload(step + 2)
                    if step < NT:
                        lt = step
                        gt = base_gt + lt
                        ti = lt % TPS
                        stageA(step + 1, None, 1 + ti, gt=gt, part="early")
                        stageA(step + 1, None, 1 + ti, gt=gt, part="late")
                    if 1 <= step < NT + 1:
                        lt = step - 1
                        ti = lt % TPS
                        stageB(step, base_gt + lt, first=(ti == 0))
                    if step >= 2:
                        lt = step - 2
                        stageC(step - 1, lt)
                    for zi in range(3 * step, min(3 * step + 3, NSLOT // 128)):
                        T.seg(2 * (zi % 3), 6)
                        T.op("act", lambda e, zi=zi: e.dma_start(out=mslot[zi * 128:(zi + 1) * 128, :], in_=zt[:]),
                             r=["zt"], dma="Z_%d" % (zi % 4))
                    k0 = 3 * step
                    for i_, sl in enumerate((1, 3, 5)):
                        if k0 + i_ < 6 * NE:
                            T.seg(sl, 4)
                            conv_cast(k0 + i_)
                    for kk, sl in ((k0 + 1, 1), (k0 + 2, 3), (k0 + 3, 5)):
                        if kk < 6 * NE:
                            T.seg(sl, 5)
                            conv_load(kk)
                    T.end_step()
                T.flush()

        def dispatch():
            PFX = "dsp"
            with contextlib.ExitStack() as s1:
                def sb1(name, shape, dt=F32):
                    return s1.enter_context(nc.sbuf_tensor("%s_%s" % (PFX, name), list(shape), dt))

                def ps1(name, shape, dt=F32):
                    return s1.enter_context(nc.psum_tensor("%s_%s" % (PFX, name), list(shape), dt))

                rankb = ps1("rankb", [128, NTT * 16])
                cntb = ps1("cntb", [128, 512])
                sc = sb1("sc", [128, 16, 16])
                sci = sb1("sci", [128, 16], I32)
                slotmat = sb1("slotmat", [128, NTT, 16])
                tmpm = sb1("tmpm", [128, NTT, 16])
                slot1f = sb1("slot1f", [128, NTT])
                slot2f = sb1("slot2f", [128, NTT])
                ucmp = sb1("ucmp", [128, NU, 16])
                uio_i = sb1("uio_i", [128, NU, 16], I32)
                uio = sb1("uio", [128, NU, 16])
                euf = sb1("euf", [128, NU])
                pio_i = sb1("pio_i", [128, 1], I32)
                pio = sb1("pio", [128, 1])
                mt = [sb1("mt%d" % i, [128, 1024], BF16) for i in range(8)]

                def Vq(fn, r=(), w=()):
                    T.op("dve", fn, r=r, w=w)

                def Pq(fn, r=(), w=()):
                    T.op("pool", fn, r=r, w=w)

                Pq(lambda e: e.memset(Acum[:, 0, :], 0.0), w=["Acum"])
                for i in range(NTT):
                    Vq(lambda e, i=i: e.tensor_tensor(out=Acum[:, i + 1, :], in0=Acum[:, i, :], in1=Aall[:, i, :], op=ALU.add),
                       r=["Acum"], w=["Acum"])

                def rk(e):
                    for i in range(NTT):
                        e.matmul(rankb[:, i * 16:(i + 1) * 16], lhsT=tri[:], rhs=Aall[:, i, :], start=True, stop=False)
                        ins = e.matmul(rankb[:, i * 16:(i + 1) * 16], lhsT=ones128[:], rhs=Acum[:, i, :], start=False, stop=True)
                    return ins
                T.op("pe", rk, r=["Acum", "tri", "ones128"], w=["rankb"])
                T.op("pe", lambda e: e.matmul(cntb[:, 0:16], lhsT=ones128[:], rhs=Acum[:, NTT, :], start=True, stop=True),
                     r=["Acum", "ones128"], w=["cntb"])
                cnt = sc[:, 0, :]
                nuf = sc[:, 1, :]
                cum = [sc[:, 2, :], sc[:, 3, :]]
                base = sc[:, 4, :]
                k = "sc"

                def SV(fn):
                    Vq(fn, r=[k], w=[k])
                Vq(lambda e: e.tensor_scalar(out=cnt, in0=cntb[:, 0:16], scalar1=255.0, scalar2=1.0 / 256, op0=ALU.add, op1=ALU.mult),
                   r=["cntb"], w=[k])
                SV(lambda e: e.tensor_scalar(out=cnt, in0=cnt, scalar1=-0.499, scalar2=None, op0=ALU.add))
                Vq(lambda e: e.tensor_copy(out=sci[:], in_=cnt), r=[k], w=["sci"])
                Vq(lambda e: e.tensor_copy(out=nuf, in_=sci[:]), r=["sci"], w=[k])
                SV(lambda e: e.tensor_copy(out=cum[0], in_=nuf))
                src = 0
                for sh in (1, 2, 4, 8):
                    a_, b_ = cum[src], cum[1 - src]
                    SV(lambda e, a_=a_, b_=b_, sh=sh: e.tensor_copy(out=b_[:, 0:sh], in_=a_[:, 0:sh]))
                    SV(lambda e, a_=a_, b_=b_, sh=sh: e.tensor_tensor(out=b_[:, sh:16], in0=a_[:, sh:16], in1=a_[:, 0:16 - sh], op=ALU.add))
                    src = 1 - src
                cumu = cum[src]
                SV(lambda e: e.tensor_tensor(out=base, in0=cumu, in1=nuf, op=ALU.subtract))
                SV(lambda e: e.tensor_scalar(out=base, in0=base, scalar1=256.0, scalar2=None, op0=ALU.mult))
                Vq(lambda e: e.tensor_tensor(out=slotmat[:], in0=rankb[:].rearrange("p (i e) -> p i e", e=16),
                                             in1=base.unsqueeze(1).to_broadcast([128, NTT, 16]), op=ALU.add),
                   r=["rankb", k], w=["slotmat"])
                for Eall, sf, si, nm in ((E1all, slot1f, slot1i, "1"), (E2all, slot2f, slot2i, "2")):
                    Vq(lambda e, Eall=Eall: e.tensor_tensor(out=tmpm[:], in0=slotmat[:], in1=Eall[:], op=ALU.mult),
                       r=["slotmat"], w=["tmpm"])
                    Vq(lambda e, sf=sf: e.tensor_reduce(out=sf[:], in_=tmpm[:], axis=AX.X, op=ALU.add), r=["tmpm"], w=["sf" + nm])
                    Vq(lambda e, sf=sf, si=si: e.tensor_copy(out=si[:], in_=sf[:]), r=["sf" + nm], w=["si" + nm])
                Pq(lambda e: e.iota(uio_i[:], pattern=[[1, NU], [0, 16]], base=0, channel_multiplier=0), w=["uio_i"])
                Pq(lambda e: e.iota(pio_i[:], pattern=[[0, 1]], base=0, channel_multiplier=1), w=["pio_i"])
                Vq(lambda e: e.tensor_copy(out=uio[:], in_=uio_i[:]), r=["uio_i"], w=["uio"])
                Vq(lambda e: e.tensor_copy(out=pio[:], in_=pio_i[:]), r=["pio_i"], w=["pio"])
                Vq(lambda e: e.tensor_tensor(out=ucmp[:], in0=cumu.unsqueeze(1).to_broadcast([128, NU, 16]), in1=uio[:], op=ALU.is_le),
                   r=[k, "uio"], w=["ucmp"])
                Vq(lambda e: e.tensor_reduce(out=euf[:], in_=ucmp[:], axis=AX.X, op=ALU.add), r=["ucmp"], w=["euf"])
                Vq(lambda e: e.tensor_scalar(out=euf[:], in0=euf[:], scalar1=128.0, scalar2=pio[:], op0=ALU.mult, op1=ALU.add),
                   r=["euf", "pio"], w=["euf"])
                Vq(lambda e: e.tensor_copy(out=widx[:], in_=euf[:]), r=["euf"], w=["widx"])
                for i in range(NTT):
                    s_ = i % 8
                    km = "mt%d" % s_
                    T.op("sp", lambda e, i=i, s_=s_: e.dma_start(out=mt[s_][:], in_=mscr[i * 128:(i + 1) * 128, :]),
                         w=[km], dma="D_" + km)
                    for si, nm in ((slot1i, "1"), (slot2i, "2")):
                        T.op("pool", lambda e, i=i, s_=s_, si=si: e.indirect_dma_start(
                            out=mslot, out_offset=bass.IndirectOffsetOnAxis(ap=si[:, i:i + 1], axis=0),
                            in_=mt[s_][:, :], in_offset=None),
                            r=[km, "si" + nm], dma="X_%s_%d" % (nm, s_))
                T.flush()

        def experts():
            PFX = "exp"
            with contextlib.ExitStack() as s1:
                def sb1(name, shape, dt=F32):
                    return s1.enter_context(nc.sbuf_tensor("%s_%s" % (PFX, name), list(shape), dt))

                def ps1(name, shape, dt=F32):
                    return s1.enter_context(nc.psum_tensor("%s_%s" % (PFX, name), list(shape), dt))

                wbuf = [sb1("wbuf%d" % i, [128, WROW], BF16) for i in range(4)]
                mtok = [sb1("mtok%d" % i, [128, 2, 1024], BF16) for i in range(5)]
                mTt = [sb1("mTt%d" % i, [128, 8, 128], BF16) for i in range(3)]
                sg = [sb1("sg%d" % i, [128, 256]) for i in range(2)]
                hid = [sb1("hid%d" % i, [128, 256], BF16) for i in range(3)]
                hidT = [sb1("hidT%d" % i, [128, 2, 128], BF16) for i in range(2)]
                ybuf = [sb1("ybuf%d" % i, [128, 1024]) for i in range(3)]
                tp = [ps1("tp%d" % i, [128, 1024], BF16) for i in range(2)]
                hT = ps1("hT", [128, 1024], BF16)
                gu = [ps1("gu%d" % i, [128, 512]) for i in range(2)]
                yps = [ps1("yps%d" % i, [128, 512]) for i in range(3)]
                ycnt = [0]

                def loads(u):
                    p2 = u % 4
                    p3 = u % 5
                    kwb = dict(bounds_check=NE * 128 - 1, oob_is_err=False) if u >= 1 else {}
                    T.op("pool", lambda e, u=u, p2=p2, kwb=kwb: e.indirect_dma_start(
                        out=wbuf[p2][:, :], out_offset=None, in_=wscr,
                        in_offset=bass.IndirectOffsetOnAxis(ap=widx[:, u:u + 1], axis=0), **kwb),
                        w=["wbuf%d" % p2], dma="G_wbuf%d" % p2)
                    T.op("sp", lambda e, u=u, p3=p3: e.dma_start(
                        out=mtok[p3][:], in_=mslot[u * 256:(u + 1) * 256, :].rearrange("(j p) d -> p j d", p=128)),
                        w=["mtok%d" % p3], dma="D_mtok%d" % p3)

                def X1f(t):
                    u, j = divmod(t, 2)
                    p3, t3 = u % 5, t % 3
                    kmk, kmt = "mtok%d" % p3, "mTt%d" % t3
                    tpb, ktp = tp[t % 2], "tp%d" % (t % 2)

                    def tr(e):
                        for k in range(8):
                            ins = e.transpose(out=tpb[:, k * 128:(k + 1) * 128], in_=mtok[p3][:, j, k * 128:(k + 1) * 128],
                                              identity=ident[:])
                        return ins
                    T.op("pe", tr, r=[kmk, "ident"], w=[ktp])
                    T.op("dve", lambda e: e.tensor_copy(out=mTt[t3][:].rearrange("p k t -> p (k t)"), in_=tpb[:]),
                         r=[ktp], w=[kmt])

                def X2f(t):
                    u, j = divmod(t, 2)
                    p2, t2, t3 = u % 4, t % 2, t % 3
                    kw, kmt = "wbuf%d" % p2, "mTt%d" % t3

                    def mgu(e):
                        for k in range(8):
                            ins = e.matmul(gu[t2][:], lhsT=mTt[t3][:, k, :], rhs=wbuf[p2][:, k * 512:(k + 1) * 512],
                                           start=(k == 0), stop=(k == 7))
                        return ins
                    T.op("pe", mgu, r=[kmt, kw], w=["gu%d" % t2])
                    T.op("act", lambda e: e.activation(out=sg[t2][:], in_=gu[t2][:, 0:256], func=AF.Silu),
                         r=["gu%d" % t2], w=["sg%d" % t2])
                    T.op("dve", lambda e: e.tensor_tensor(out=hid[t3][:], in0=gu[t2][:, 256:512], in1=sg[t2][:], op=ALU.mult),
                         r=["gu%d" % t2, "sg%d" % t2], w=["hid%d" % t3])

                def Yf(t):
                    u, j = divmod(t, 2)
                    p2, t2, t3_ = u % 4, t % 2, t % 3
                    kw = "wbuf%d" % p2

                    def th(e):
                        for fc in range(2):
                            ins = e.transpose(out=hT[:, fc * 128:(fc + 1) * 128], in_=hid[t3_][:, fc * 128:(fc + 1) * 128],
                                              identity=ident[:])
                        return ins
                    T.op("pe", th, r=["hid%d" % t3_, "ident"], w=["hT"])
                    T.op("act", lambda e: e.activation(out=hidT[t2][:].rearrange("p f t -> p (f t)"), in_=hT[:, 0:256],
                                                       func=AF.Copy), r=["hT"], w=["hidT%d" % t2])
                    T.seg(2, 0)
                    for nh in range(2):
                        yi = ycnt[0] % 3
                        ycnt[0] += 1

                        def dn(e, nh=nh, yi=yi):
                            for fc in range(2):
                                ins = e.matmul(yps[yi][:], lhsT=hidT[t2][:, fc, :],
                                               rhs=wbuf[p2][:, 4096 + fc * 1024 + nh * 512:4096 + fc * 1024 + (nh + 1) * 512],
                                               start=(fc == 0), stop=(fc == 1))
                            return ins
                        T.op("pe", dn, r=["hidT%d" % t2, kw], w=["yps%d" % yi])
                        T.op("act", lambda e, nh=nh, yi=yi: e.activation(out=ybuf[t3_][:, nh * 512:(nh + 1) * 512], in_=yps[yi][:],
                                                                         func=AF.Copy),
                             r=["yps%d" % yi], w=["ybuf%d_%d" % (t3_, nh)])
                    T.op("act", lambda e: e.dma_start(out=yscr[t * 128:(t + 1) * 128, :], in_=ybuf[t3_][:]),
                         r=["ybuf%d_0" % t3_, "ybuf%d_1" % t3_], w=[], dma="S_ybuf%d" % t3_)

                loads(0)
                loads(1)
                loads(2)
                loads(3)
                NTL = 2 * NU
                X1f(0)
                X1f(1)
                X2f(0)
                X1f(2)
                X2f(1)
                for t in range(NTL):
                    T.seg(0, 0)
                    Yf(t)
                    if t + 2 < NTL:
                        T.seg(1, 1)
                        X2f(t + 2)
                    if t + 3 < NTL:
                        T.seg(3, 2)
                        X1f(t + 3)
                    T.end_step()
                    if t % 2 == 1 and (t - 1) // 2 + 4 < NU:
                        loads((t - 1) // 2 + 4)
                T.flush()

        def combine():
            PFX = "cmb"
            with contextlib.ExitStack() as s1:
                def sb1(name, shape, dt=F32):
                    return s1.enter_context(nc.sbuf_tensor("%s_%s" % (PFX, name), list(shape), dt))

                ya = [sb1("ya%d" % i, [128, 1024]) for i in range(3)]
                yb_ = [sb1("yb%d" % i, [128, 1024]) for i in range(3)]
                hb = [sb1("hb%d" % i, [128, 1024]) for i in range(3)]
                acc = [sb1("acc%d" % i, [128, 1024]) for i in range(3)]
                for i in range(NTT):
                    s_ = i % 3
                    T.op("pool", lambda e, i=i, s_=s_: e.indirect_dma_start(
                        out=ya[s_][:, :], out_offset=None, in_=yscr,
                        in_offset=bass.IndirectOffsetOnAxis(ap=slot1i[:, i:i + 1], axis=0)), w=["ya%d" % s_], dma="G_ya%d" % s_)
                    T.op("pool", lambda e, i=i, s_=s_: e.indirect_dma_start(
                        out=yb_[s_][:, :], out_offset=None, in_=yscr,
                        in_offset=bass.IndirectOffsetOnAxis(ap=slot2i[:, i:i + 1], axis=0)), w=["yb%d" % s_], dma="G_yb%d" % s_)
                    T.op("sp", lambda e, i=i, s_=s_: e.dma_start(out=hb[s_][:], in_=out[i * 128:(i + 1) * 128, :]),
                         w=["hb%d" % s_], dma="D_hb%d" % s_)
                    T.op("dve", lambda e, i=i, s_=s_: e.scalar_tensor_tensor(
                        out=acc[s_][:], in0=ya[s_][:], scalar=w12[:, i, 0:1], in1=hb[s_][:], op0=ALU.mult, op1=ALU.add),
                        r=["ya%d" % s_, "hb%d" % s_], w=["acc%d" % s_])
                    T.op("dve", lambda e, i=i, s_=s_: e.scalar_tensor_tensor(
                        out=acc[s_][:], in0=yb_[s_][:], scalar=w12[:, i, 1:2], in1=acc[s_][:], op0=ALU.mult, op1=ALU.add),
                        r=["yb%d" % s_, "acc%d" % s_], w=["acc%d" % s_])
                    T.op("act", lambda e, i=i, s_=s_: e.dma_start(out=out[i * 128:(i + 1) * 128, :], in_=acc[s_][:]),
                         r=["acc%d" % s_], dma="S_acc%d" % s_)
                T.flush()

        for rnd in range(NR):
            phase1(rnd, do_meta=(rnd == 0))
        dispatch()
        experts()
        combine()
    return nc


_NC_CACHE = {}


def kernel(x, meta_tokens, attn_norm_gain, w_in, w_pool, pool_scale, q_norm_gain, k_norm_gain,
           attn_sinks, w_out, ffn_norm_gain, w_group_router, w_expert_router, w_gate, w_up, w_down):
    f = lambda a: np.ascontiguousarray(np.asarray(a, dtype=np.float32))
    x = f(x)
    B = x.shape[0]
    ncores = 8
    per = B // ncores
    w_in0 = f(w_in)[0]
    perm = []
    for b in range(4):
        for gsel in range(2):
            h = gsel * 4 + b
            perm.extend(range(512 + h * 64, 512 + (h + 1) * 64))
    cols = list(range(512)) + perm + list(range(1024, 1280))
    w_in_p = np.ascontiguousarray(w_in0[:, cols])
    shared = {
        "meta": f(meta_tokens),
        "ag": f(attn_norm_gain).reshape(1, D),
        "w_in": w_in_p,
        "w_pool": f(w_pool)[0],
        "pscale": f(pool_scale).reshape(1, 512),
        "qg": f(q_norm_gain).reshape(1, 64),
        "kg": f(k_norm_gain).reshape(1, 64),
        "sinks": f(attn_sinks).reshape(1, 8),
        "w_out": f(w_out)[0],
        "fg": f(ffn_norm_gain).reshape(1, D),
        "wgr": f(w_group_router)[0],
        "wer": f(w_expert_router)[0],
        "w_gate": f(w_gate)[0],
        "w_up": f(w_up)[0],
        "w_down": f(w_down)[0],
    }
    if "nc" not in _NC_CACHE:
        _NC_CACHE["nc"] = build_nc()
    nc = _NC_CACHE["nc"]
    in_maps = []
    for c in range(ncores):
        m = dict(shared)
        m["x"] = np.ascontiguousarray(x[c * per:(c + 1) * per].reshape(per * SEQ, D))
        in_maps.append(m)
    res = run_bass_kernel_spmd(nc, in_maps, core_ids=list(range(ncores)))
    outs = [np.asarray(r["out"], dtype=np.float32).reshape(per, SEQ, D) for r in res.results]
    return np.concatenate(outs, axis=0)
```

```python
import math
import contextlib
import numpy as np
import concourse.bass as bass
import concourse.mybir as mybir
from concourse.bass_utils import run_bass_kernel_spmd

F32 = mybir.dt.float32
BF16 = mybir.dt.bfloat16
I32 = mybir.dt.int32
ALU = mybir.AluOpType
AF = mybir.ActivationFunctionType
AX = mybir.AxisListType

D = 1024
SEQ = 2048
NSEQ = 2
NT = 32
TB = NT * 128
NR = NSEQ * SEQ // TB
NE = 16
NTT = NSEQ * SEQ // 128
NU = 48
NSLOT = NU * 256
WROW = 8 * 512 + 2 * 1024
POOLW = (2, 4, 8, 16)
EPS = 1e-6
THETA = 500000.0
INV_FREQ = [1.0 / (THETA ** (i / 8.0)) for i in range(8)]


class Op:
    __slots__ = ("eng", "fn", "r", "w", "dma", "deps", "sig", "sigval", "blk")


class Tracker:
    def __init__(self, nc, stack):
        self.nc = nc
        self.stack = stack
        self.ops = []
        self.lastw = {}
        self.readers = {}
        self.sems = {}
        self.cnt = {}
        self.waited = {}
        self.blk = 0
        self.cur = None
        self.noseg = False
        self.step_ops = {}

    def seg(self, slot, stage):
        if not self.noseg:
            self.cur = (slot, stage)

    def end_step(self):
        so = self.step_ops
        self.step_ops = {}
        self.cur = None
        for slot in sorted({k[0] for k in so}):
            items = []
            for k in sorted(so):
                if k[0] != slot:
                    continue
                lst = so[k]
                for i, o in enumerate(lst):
                    items.append(((i + 0.5) / len(lst), k[1], i, o))
            items.sort(key=lambda x: (x[0], x[1], x[2]))
            self.ops.extend(o for _, _, _, o in items)

    def sem(self, key):
        if key not in self.sems:
            self.sems[key] = self.stack.enter_context(self.nc.semaphore("s_" + key))
            self.cnt[key] = 0
        return self.sems[key]

    def op(self, eng, fn, r=(), w=(), dma=None):
        o = Op()
        o.eng, o.fn, o.r, o.w, o.dma = eng, fn, tuple(r), tuple(w), dma
        o.deps, o.sig, o.sigval, o.blk = [], dma is not None, 0, self.blk
        if self.cur is not None:
            self.step_ops.setdefault(self.cur, []).append(o)
        else:
            self.ops.append(o)
        return o

    def flush(self):
        ops, self.ops = self.ops, []
        if not ops:
            return
        for o in ops:
            deps = []
            for b in o.r:
                lw = self.lastw.get(b)
                if lw is not None:
                    deps.append(lw)
            for b in o.w:
                lw = self.lastw.get(b)
                if lw is not None:
                    deps.append(lw)
                deps.extend(self.readers.get(b, ()))
            for b in o.r:
                self.readers.setdefault(b, []).append(o)
            for b in o.w:
                self.lastw[b] = o
                self.readers[b] = []
            seen = set()
            for d in deps:
                if d is o or id(d) in seen or d.blk != self.blk:
                    continue
                seen.add(id(d))
                if d.dma is None and o.dma is None and d.eng == "pe" and o.eng == "pe":
                    continue
                o.deps.append(d)
                d.sig = True
        for o in ops:
            if o.sig:
                key = o.dma or o.eng
                self.sem(key)
                self.cnt[key] += 16 if o.dma else 1
                o.sigval = self.cnt[key]
        engs = []
        for o in ops:
            if o.eng not in engs:
                engs.append(o.eng)
        reg = {"pe": "tensor", "act": "scalar", "dve": "vector", "pool": "gpsimd", "sp": "sync"}
        with self.nc.Block() as blk:
            for eng in engs:
                def body(e, eng=eng):
                    dmakeys = []
                    for o in ops:
                        if o.eng != eng:
                            continue
                        for d in o.deps:
                            key = d.dma or d.eng
                            if self.waited.get((eng, key), 0) < d.sigval:
                                e.wait_ge(self.sems[key], d.sigval)
                                self.waited[(eng, key)] = d.sigval
                        ins = o.fn(e)
                        if o.sig:
                            ins.then_inc(self.sems[o.dma or o.eng], 16 if o.dma else 1)
                        if o.dma and o.dma not in dmakeys:
                            dmakeys.append(o.dma)
                    for key in dmakeys:
                        if self.waited.get((eng, key), 0) < self.cnt[key]:
                            e.wait_ge(self.sems[key], self.cnt[key])
                            self.waited[(eng, key)] = self.cnt[key]
                getattr(blk, reg[eng])(body)
        self.blk += 1


def build_nc():
    nc = bass.Bass("TRN2", target_bir_lowering=False)
    dr = {}

    def din(name, shape):
        dr[name] = nc.dram_tensor(name, list(shape), F32, kind="ExternalInput").ap()

    din("x", (NSEQ * SEQ, D))
    din("meta", (16, D))
    din("ag", (1, D))
    din("w_in", (D, 1280))
    din("w_pool", (4, 128, 128))
    din("pscale", (1, 512))
    din("qg", (1, 64))
    din("kg", (1, 64))
    din("sinks", (1, 8))
    din("w_out", (D, D))
    din("fg", (1, D))
    din("wgr", (D, 4))
    din("wer", (D, 16))
    din("w_gate", (NE, D, 256))
    din("w_up", (NE, D, 256))
    din("w_down", (NE, 256, D))
    out = nc.dram_tensor("out", [NSEQ * SEQ, D], F32, kind="ExternalOutput").ap()
    mscr = nc.dram_tensor("mscr", [NSEQ * SEQ, D], BF16, kind="Internal").ap()
    mslot = nc.dram_tensor("mslot", [NSLOT, D], BF16, kind="Internal").ap()
    yscr = nc.dram_tensor("yscr", [NSLOT, D], F32, kind="Internal").ap()
    wscr = nc.dram_tensor("wscr", [NE * 128, WROW], BF16, kind="Internal").ap()

    with contextlib.ExitStack() as st:
        T = Tracker(nc, st)

        def sb(name, shape, dt=F32):
            return st.enter_context(nc.sbuf_tensor(name, list(shape), dt))

        win = sb("win", [128, 8, 1280], BF16)
        wop = sb("wop", [128, 4, 1024], BF16)
        woa = sb("woa", [128, 4, 1024], BF16)
        wpl = sb("wpl", [128, 4, 128], BF16)
        wr = sb("wr", [128, 8, 20], BF16)
        ag_t = sb("ag_t", [128, 8])
        fg_t = sb("fg_t", [128, 8])
        fgb = sb("fgb", [128, 1024])
        ps_t = sb("ps_t", [128, 4])
        qkgain = sb("qkgain", [128, 10, 64])
        ident = sb("ident", [128, 128], BF16)
        mask_d = sb("mask_d", [128, 128], BF16)
        mask_p = sb("mask_p", [128, 128], BF16)
        negm_d = sb("negm_d", [128, 4, 128], BF16)
        negm_p = sb("negm_p", [128, 4, 128], BF16)
        Pd = sb("Pd", [128, 4, 128], BF16)
        Pp = sb("Pp", [128, 4, 128], BF16)
        Pm = sb("Pm", [16, 4, 128], BF16)
        ones64 = sb("ones64", [128, 64], BF16)
        onesm = sb("onesm", [33, 64], BF16)
        sel = sb("sel", [16, 16, 128], BF16)
        eps_t = sb("eps_t", [128, 1])
        negC = sb("negC", [128, 1])
        cosT = sb("cosT", [128, 17, 8])
        sinT = sb("sinT", [128, 17, 8])
        um = sb("um", [128, 512], BF16)
        KTm = sb("KTm", [128, 128], BF16)
        Vm = [sb("Vm%d" % g, [33, 64], BF16) for g in range(2)]
        PTm = [[sb("PTm%d_%d" % (g, s), [33, 512], BF16) for s in range(2)] for g in range(2)]
        uring = [sb("ur%d" % i, [128, 512], BF16) for i in range(3)]
        vring = [sb("vr%d" % i, [128, 128], BF16) for i in range(3)]
        kring = [sb("kr%d" % i, [128, 128], BF16) for i in range(3)]
        tri = sb("tri", [128, 128], BF16)
        zt = sb("zt", [128, 1024], BF16)
        ones128 = sb("ones128", [128, 128], BF16)
        E1all = sb("E1all", [128, NTT, 16])
        E2all = sb("E2all", [128, NTT, 16])
        Acum = sb("Acum", [128, NTT + 1, 16], BF16)
        Aall = sb("Aall", [128, NTT, 16], BF16)
        w12 = sb("w12", [128, NTT, 2])
        slot1i = sb("slot1i", [128, NTT], I32)
        slot2i = sb("slot2i", [128, NTT], I32)
        widx = sb("widx", [128, NU], I32)

        with contextlib.ExitStack() as s1:
            def sb1(name, shape, dt=F32):
                return s1.enter_context(nc.sbuf_tensor(name, list(shape), dt))

            def ps1(name, shape, dt=F32):
                return s1.enter_context(nc.psum_tensor(name, list(shape), dt))

            stg = [sb1("stg%d" % i, [128, 1280]) for i in range(4)]
            qg_b = sb1("qg_b", [128, 64])
            kg_b = sb1("kg_b", [128, 64])
            sk = sb1("sk", [128, 8])
            es = sb1("es", [128, 8])
            identf = sb1("identf", [128, 128])
            Pf = sb1("Pf", [128, 12, 128])
            posi = sb1("posi", [128, 17], I32)
            posf = sb1("posf", [128, 17])
            ang = sb1("ang", [128, 17, 8])
            angi = sb1("angi", [128, 17, 8], I32)
            angf = sb1("angf", [128, 17, 8])
            tA = sb1("tA", [128, 17, 8])
            tB = sb1("tB", [128, 17, 8])
            mq = sb1("mq", [128, 1])
            mk = sb1("mk", [128, 1])
            wrs = sb1("wrs", [128, 8, 20])

            def ld(dst, src, key, buf, **kw):
                T.op("sp", lambda e: e.dma_start(out=dst, in_=src, **kw), w=[buf], dma=key)

            ld(ag_t[:], dr["ag"].rearrange("o (c p) -> p (o c)", p=128), "L_ag", "ag_t", allow_slow_non_contiguous=True)
            ld(fg_t[:], dr["fg"].rearrange("o (c p) -> p (o c)", p=128), "L_fg", "fg_t", allow_slow_non_contiguous=True)
            ld(ps_t[:], dr["pscale"].rearrange("o (g d) -> d (o g)", d=128), "L_ps", "ps_t", allow_slow_non_contiguous=True)
            ld(qg_b[:], dr["qg"].partition_broadcast(128), "L_qg", "qg_b")
            ld(fgb[:], dr["fg"].partition_broadcast(128), "L_fgb", "fgb")
            ld(kg_b[:], dr["kg"].partition_broadcast(128), "L_kg", "kg_b")
            ld(sk[32:33, :], dr["sinks"], "L_sk", "sk")
            ld(wrs[:, :, 0:4], dr["wgr"].rearrange("(c p) n -> p c n", p=128), "L_wgr", "wrs_a")
            ld(wrs[:, :, 4:20], dr["wer"].rearrange("(c p) n -> p c n", p=128), "L_wer", "wrs_b")

            sidx = [0]
            NSTG = 4

            def stage_load(parts, consume):
                s_ = sidx[0] % NSTG
                sidx[0] += 1
                key = "stg%d" % s_
                for dst_fn, src_ap in parts:
                    T.op("sp", lambda e, dst_fn=dst_fn, src_ap=src_ap, s_=s_: e.dma_start(out=dst_fn(stg[s_]), in_=src_ap),
                         w=[key], dma="L_%s_%d" % (key, len(parts)))
                consume(stg[s_], key)

            for c in range(8):
                def cons(t, key, c=c):
                    T.op("act", lambda e: e.activation(out=win[:, c, :], in_=t[:, 0:1280], func=AF.Copy, scale=ag_t[:, c:c + 1]),
                         r=[key, "ag_t"], w=["win%d" % c])
                stage_load([(lambda t: t[:, 0:1280], dr["w_in"][c * 128:(c + 1) * 128, :])], cons)
            for c in range(4):
                def cons(t, key, c=c):
                    T.op("act", lambda e: e.activation(out=wop[:, c, :], in_=t[:, 0:1024], func=AF.Copy), r=[key], w=["wop%d" % c])
                stage_load([(lambda t: t[:, 0:1024], dr["w_out"][c * 128:(c + 1) * 128, :])], cons)
            for hl in range(4):
                def cons(t, key, hl=hl):
                    T.op("act", lambda e: e.activation(out=woa[:, hl, :], in_=t[:, 0:1024], func=AF.Copy), r=[key], w=["woa%d" % hl])
                stage_load([(lambda t: t[0:64, 0:1024], dr["w_out"][512 + hl * 64:512 + (hl + 1) * 64, :]),
                            (lambda t: t[64:128, 0:1024], dr["w_out"][512 + (4 + hl) * 64:512 + (5 + hl) * 64, :])], cons)

            def cons(t, key):
                T.op("act", lambda e: e.activation(out=wpl[:].rearrange("p g d -> p (g d)"), in_=t[:, 0:512], func=AF.Copy),
                     r=[key], w=["wpl"])
            stage_load([(lambda t: t[:, 0:512].rearrange("p (g d) -> p g d", g=4), dr["w_pool"].rearrange("g c d -> c g d"))], cons)

            def P(fn, r=(), w=()):
                T.op("pool", fn, r=r, w=w)

            def V(fn, r=(), w=()):
                T.op("dve", fn, r=r, w=w)

            def A(fn, r=(), w=()):
                T.op("act", fn, r=r, w=w)

            P(lambda e: e.memset(eps_t[:], EPS), w=["eps_t"])
            P(lambda e: e.memset(identf[:], 1.0), w=["identf"])
            P(lambda e: e.affine_select(out=identf[:], in_=identf[:], pattern=[[-1, 128]], compare_op=ALU.is_equal,
                                        fill=0.0, base=0, channel_multiplier=1), r=["identf"], w=["identf"])
            V(lambda e: e.tensor_copy(out=ident[:], in_=identf[:]), r=["identf"], w=["ident"])
            P(lambda e: e.memset(mask_d[:], 1.0), w=["mask_d"])
            P(lambda e: e.affine_select(out=mask_d[:], in_=mask_d[:], pattern=[[1, 128]], compare_op=ALU.is_ge,
                                        fill=0.0, base=0, channel_multiplier=-1), r=["mask_d"], w=["mask_d"])
            P(lambda e: e.memset(mask_p[:], 1.0), w=["mask_p"])
            P(lambda e: e.affine_select(out=mask_p[:], in_=mask_p[:], pattern=[[-1, 128]], compare_op=ALU.is_gt,
                                        fill=0.0, base=0, channel_multiplier=1), r=["mask_p"], w=["mask_p"])
            for mk_, ng_, nm_ in ((mask_d, negm_d, "negm_d"), (mask_p, negm_p, "negm_p")):
                V(lambda e, mk_=mk_, ng_=ng_: e.tensor_scalar(out=ng_[:], in0=mk_[:].unsqueeze(1).to_broadcast([128, 4, 128]),
                                                             scalar1=-1.0, scalar2=30000.0, op0=ALU.add, op1=ALU.mult),
                  r=["mask_d", "mask_p"], w=[nm_])
            P(lambda e: e.memset(ones64[:], 1.0), w=["ones64"])
            P(lambda e: e.memset(ones128[:], 1.0), w=["ones128"])
            P(lambda e: e.memset(zt[:], 0.0), w=["zt"])
            P(lambda e: e.memset(tri[:], 1.0), w=["tri"])
            P(lambda e: e.affine_select(out=tri[:], in_=tri[:], pattern=[[1, 128]], compare_op=ALU.is_gt,
                                        fill=0.0, base=0, channel_multiplier=-1), r=["tri"], w=["tri"])
            P(lambda e: e.memset(onesm[:], 0.0), w=["onesm"])
            P(lambda e: e.memset(onesm[0:16, :], 1.0), r=["onesm"], w=["onesm"])
            P(lambda e: e.memset(onesm[32:33, :], 1.0), r=["onesm"], w=["onesm"])
            P(lambda e: e.memset(sel[:], 1.0), w=["sel"])
            P(lambda e: e.affine_select(out=sel[:], in_=sel[:], pattern=[[1, 16], [0, 128]], compare_op=ALU.is_equal,
                                        fill=0.0, base=0, channel_multiplier=-1), r=["sel"], w=["sel"])
            for g, w_ in enumerate(POOLW):
                iw = 1.0 / w_
                k = "Pf%d" % g
                P(lambda e, g=g, iw=iw: e.memset(Pf[:, g, :], iw), w=[k])
                P(lambda e, g=g: e.affine_select(out=Pf[:, g, :], in_=Pf[:, g, :], pattern=[[1, 128]], compare_op=ALU.is_ge,
                                                 fill=0.0, base=0, channel_multiplier=-1), r=[k], w=[k])
                P(lambda e, g=g, w_=w_: e.affine_select(out=Pf[:, g, :], in_=Pf[:, g, :], pattern=[[-1, 128]], compare_op=ALU.is_ge,
                                                        fill=0.0, base=w_ - 1, channel_multiplier=1), r=[k], w=[k])
                V(lambda e, g=g: e.tensor_tensor(out=Pd[:, g, :], in0=Pf[:, g, :], in1=identf[:], op=ALU.subtract),
                  r=[k, "identf"], w=["Pd"])
                k2 = "Pf%d" % (4 + g)
                P(lambda e, g=g, iw=iw: e.memset(Pf[:, 4 + g, :], iw), w=[k2])
                P(lambda e, g=g, w_=w_: e.affine_select(out=Pf[:, 4 + g, :], in_=Pf[:, 4 + g, :], pattern=[[-1, 128]],
                                                        compare_op=ALU.is_ge, fill=0.0, base=w_ - 129, channel_multiplier=1),
                  r=[k2], w=[k2])
                V(lambda e, g=g: e.tensor_copy(out=Pp[:, g, :], in_=Pf[:, 4 + g, :]), r=[k2], w=["Pp"])
                k3 = "Pf%d" % (8 + g)
                P(lambda e, g=g, iw=iw: e.memset(Pf[0:16, 8 + g, :], iw), w=[k3])
                P(lambda e, g=g, w_=w_: e.affine_select(out=Pf[0:16, 8 + g, :], in_=Pf[0:16, 8 + g, :], pattern=[[-1, 128]],
                                                        compare_op=ALU.is_ge, fill=0.0, base=w_ - 17, channel_multiplier=1),
                  r=[k3], w=[k3])
                V(lambda e, g=g: e.tensor_copy(out=Pm[:, g, :], in_=Pf[0:16, 8 + g, :]), r=[k3], w=["Pm"])
            P(lambda e: e.iota(posi[:, 1:17], pattern=[[128, 16]], base=16, channel_multiplier=1), w=["posi_a"])
            P(lambda e: e.iota(posi[:, 0:1], pattern=[[0, 1]], base=0, channel_multiplier=1), w=["posi_b"])
            V(lambda e: e.tensor_copy(out=posf[:], in_=posi[:]), r=["posi_a", "posi_b"], w=["posf"])
            for i in range(8):
                V(lambda e, i=i: e.tensor_scalar(out=ang[:, :, i], in0=posf[:], scalar1=INV_FREQ[i] / (2 * math.pi),
                                                 scalar2=None, op0=ALU.mult), r=["posf"], w=["ang"])

            def trig(dst, shift, nm):
                V(lambda e: e.tensor_scalar(out=tA[:], in0=ang[:], scalar1=shift, scalar2=None, op0=ALU.add),
                  r=["ang"], w=["tA"])
                V(lambda e: e.tensor_copy(out=angi[:], in_=tA[:]), r=["tA"], w=["angi"])
                V(lambda e: e.tensor_copy(out=angf[:], in_=angi[:]), r=["angi"], w=["angf"])
                V(lambda e: e.tensor_tensor(out=tA[:], in0=tA[:], in1=angf[:], op=ALU.subtract), r=["tA", "angf"], w=["tA"])
                V(lambda e: e.tensor_scalar(out=tB[:], in0=tA[:], scalar1=0.5, scalar2=None, op0=ALU.is_gt), r=["tA"], w=["tB"])
                V(lambda e: e.tensor_tensor(out=tA[:], in0=tA[:], in1=tB[:], op=ALU.subtract), r=["tA", "tB"], w=["tA"])
                V(lambda e: e.tensor_scalar(out=tB[:], in0=tA[:], scalar1=-0.5, scalar2=None, op0=ALU.is_lt), r=["tA"], w=["tB"])
                V(lambda e: e.tensor_tensor(out=tA[:], in0=tA[:], in1=tB[:], op=ALU.add), r=["tA", "tB"], w=["tA"])
                A(lambda e: e.activation(out=dst[:], in_=tA[:], func=AF.Sin, scale=2 * math.pi), r=["tA"], w=[nm])

            trig(sinT, 0.0, "sinT")
            trig(cosT, 0.25, "cosT")
            V(lambda e: e.tensor_copy(out=qkgain[:, 0:8, :], in_=qg_b[:].unsqueeze(1).to_broadcast([128, 8, 64])),
              r=["qg_b"], w=["qkgain_a"])
            V(lambda e: e.tensor_copy(out=qkgain[:, 8:10, :], in_=kg_b[:].unsqueeze(1).to_broadcast([128, 2, 64])),
              r=["kg_b"], w=["qkgain_b"])
            V(lambda e: e.reduce_max(out=mq[:], in_=qg_b[:], axis=AX.X, apply_absolute_value=True), r=["qg_b"], w=["mq"])
            V(lambda e: e.reduce_max(out=mk[:], in_=kg_b[:], axis=AX.X, apply_absolute_value=True), r=["kg_b"], w=["mk"])
            V(lambda e: e.tensor_tensor(out=negC[:], in0=mq[:], in1=mk[:], op=ALU.mult), r=["mq", "mk"], w=["negC"])
            V(lambda e: e.tensor_scalar(out=negC[:], in0=negC[:], scalar1=-8.0, scalar2=None, op0=ALU.mult),
              r=["negC"], w=["negC"])
            A(lambda e: e.activation(out=es[32:33, :], in_=sk[32:33, :], func=AF.Exp, bias=negC[32:33, :]),
              r=["sk", "negC"], w=["es"])
            for g in range(2):
                for s in range(2):
                    k = "PTm%d_%d" % (g, s)
                    P(lambda e, g=g, s=s: e.memset(PTm[g][s][:], 0.0), w=[k])
                    V(lambda e, g=g, s=s: e.tensor_copy(
                        out=PTm[g][s][32:33, :].rearrange("p (h q) -> p h q", h=4),
                        in_=es[32:33, 4 * g:4 * g + 4].unsqueeze(2).to_broadcast([1, 4, 128])), r=["es", k], w=[k])
            for c in range(8):
                V(lambda e, c=c: e.tensor_copy(out=wr[:, c, :], in_=wrs[:, c, :]), r=["wrs_a", "wrs_b"], w=["wr"])
            T.flush()

        def phase1(rnd, do_meta):
            PFX = "p1r%d" % rnd
            TPS = SEQ // 128
            with contextlib.ExitStack() as s1:
                def sb1(name, shape, dt=F32):
                    return s1.enter_context(nc.sbuf_tensor("%s_%s" % (PFX, name), list(shape), dt))

                def ps1(name, shape, dt=F32):
                    return s1.enter_context(nc.psum_tensor("%s_%s" % (PFX, name), list(shape), dt))

                xt = [sb1("xt%d" % i, [128, 1024]) for i in range(5)]
                junk = sb1("junk", [128, 1024], BF16)
                junkC = sb1("junkC", [128, 1024], BF16)
                xb = [sb1("xb%d" % i, [128, 1024], BF16) for i in range(2)]
                xT = [sb1("xT%d" % i, [128, 8, 128], BF16) for i in range(2)]
                qk = [sb1("qk%d" % i, [128, 640]) for i in range(2)]
                sq = sb1("sq", [128, 640])
                qn = [sb1("qn%d" % i, [128, 10, 64]) for i in range(2)]
                qf = [sb1("qf%d" % i, [128, 10, 64], BF16) for i in range(2)]
                rt = [sb1("rt%d" % i, [128, 10, 8]) for i in range(4)]
                st_ = [sb1("st%d" % i, [128, 16]) for i in range(2)]
                hs = [sb1("hs%d" % i, [128, 10]) for i in range(2)]
                hr = [sb1("hr%d" % i, [128, 10]) for i in range(2)]
                QT = [sb1("QT%d" % i, [128, 4, 128], BF16) for i in range(2)]
                PTd = [[sb1("PTd%d_%d" % (g, i), [128, 512], BF16) for i in range(2)] for g in range(2)]
                PTp = [[sb1("PTp%d_%d" % (g, i), [128, 512], BF16) for i in range(2)] for g in range(2)]
                rD = sb1("rD", [128, 512])
                yT = [sb1("yT%d" % i, [128, 512], BF16) for i in range(2)]
                mixT = [sb1("mixT%d" % i, [128, 4, 128], BF16) for i in range(2)]
                ypT = [sb1("ypT%d" % i, [128, 4, 128], BF16) for i in range(2)]
                mb = [sb1("mb%d" % i, [128, 1024], BF16) for i in range(2)]
                st2 = [sb1("st2_%d" % i, [128, 4]) for i in range(2)]
                rs = [sb1("rs%d" % i, [128, 96]) for i in range(2)]
                vm_tmp = sb1("vm_tmp", [128, 128], BF16)
                h1t = [sb1("h1t%d" % i, [128, 1024]) for i in range(2)]
                mTt = [sb1("mTt%d" % i, [128, 8, 128], BF16) for i in range(2)]
                cstg = [sb1("cstg%d" % i, [128, 1024]) for i in range(3)]
                wtile = [sb1("wtile%d" % i, [128, WROW], BF16) for i in range(2)]
                cidx = [0]

                tp = ps1("tp", [128, 1024], BF16)
                b1 = ps1("b1", [128, 512])
                b2 = ps1("b2", [128, 512])
                b3 = ps1("b3", [128, 512])
                b4 = ps1("b4", [128, 512])
                b5 = ps1("b5", [128, 512])
                b6 = ps1("b6", [128, 512])
                b7 = ps1("b7", [128, 512])

                def Pq(fn, r=(), w=()):
                    T.op("pool", fn, r=r, w=w)

                def Vq(fn, r=(), w=()):
                    T.op("dve", fn, r=r, w=w)

                def Aq(fn, r=(), w=()):
                    T.op("act", fn, r=r, w=w)

                def Eq(fn, r=(), w=()):
                    T.op("pe", fn, r=r, w=w)

                def rstd_chain(src, n_inv, dst, lnbuf, kin, kln, kout):
                    Aq(lambda e: e.activation(out=lnbuf, in_=src, func=AF.Ln, scale=n_inv, bias=eps_t[:]),
                       r=[kin, "eps_t"], w=[kln])
                    Aq(lambda e: e.activation(out=dst, in_=lnbuf, func=AF.Exp, scale=-0.5), r=[kln], w=[kout])

                def stageA(a, src_rows, c, meta=False, gt=None, part="both"):
                    s2 = a % 2
                    s3 = a % 5
                    kx = "xt%d" % s3
                    ss = st_[s2][:, 0:1]
                    lnv = st_[s2][:, 1:2]
                    rstd = st_[s2][:, 2:3]
                    kss, kln, krs = "ss%d" % s2, "lnv%d" % s2, "rstd%d" % s2
                    kxb = "xb%d" % s2
                    kxT = "xT%d" % s2
                    if part in ("both", "early"):
                        if meta:
                            Pq(lambda e: e.memset(xt[s3][:], 0.0), w=[kx])
                            T.op("sp", lambda e: e.dma_start(out=xt[s3][0:16, :], in_=dr["meta"]), r=[kx], w=[kx], dma="D_" + kx)
                        else:
                            T.seg(0, 3)
                        Aq(lambda e: e.activation(out=junk[:], in_=xt[s3][:], func=AF.Square, accum_out=ss), r=[kx], w=["junk", kss])
                        rstd_chain(ss, 1.0 / D, rstd, lnv, kss, kln, krs)
                        Vq(lambda e: e.tensor_copy(out=xb[s2][:], in_=xt[s3][:]), r=[kx], w=[kxb])

                        def t1(e):
                            for k in range(8):
                                ins = e.transpose(out=tp[:, k * 128:(k + 1) * 128], in_=xb[s2][:, k * 128:(k + 1) * 128], identity=ident[:])
                            return ins
                        Eq(t1, r=[kxb, "ident"], w=["tp"])
                        Vq(lambda e: e.tensor_copy(out=xT[s2][:].rearrange("p k t -> p (k t)"), in_=tp[:]), r=["tp"], w=[kxT])
                        if part == "early":
                            return
                    if not meta:
                        T.seg(2, 0)

                    def mm(bank, c0, c1):
                        def f(e):
                            for k in range(8):
                                ins = e.matmul(bank[:, 0:c1 - c0], lhsT=xT[s2][:, k, :], rhs=win[:, k, c0:c1],
                                               start=(k == 0), stop=(k == 7))
                            return ins
                        return f
                    Eq(mm(b1, 0, 512), r=[kxT, "win"], w=["b1_0", "b1_1"])
                    Eq(mm(b2, 512, 1024), r=[kxT, "win"], w=["b2_0", "b2_1"])
                    Eq(mm(b3, 1024, 1280), r=[kxT, "win"], w=["b3"])
                    if meta:
                        ubuf, kub = um, "um"
                        vbuf, kvb = vm_tmp, "vm_tmp"
                        kbuf, kkb = KTm, "KTm"
                    else:
                        ubuf, kub = uring[gt % 3], "ur%d" % (gt % 3)
                        vbuf, kvb = vring[gt % 3], "vr%d" % (gt % 3)
                        kbuf, kkb = kring[gt % 3], "kr%d" % (gt % 3)
                    Aq(lambda e: e.activation(out=ubuf[:], in_=b1[:], func=AF.Copy, scale=rstd), r=["b1_0", "b1_1", krs], w=[kub])
                    kqk = "qk%d" % s2
                    Aq(lambda e: e.activation(out=qk[s2][:, 0:512], in_=b2[:], func=AF.Copy, scale=rstd), r=["b2_0", "b2_1", krs], w=[kqk + "a"])
                    Aq(lambda e: e.activation(out=qk[s2][:, 512:640], in_=b3[:, 0:128], func=AF.Copy, scale=rstd),
                       r=["b3", krs], w=[kqk + "b"])
                    Aq(lambda e: e.activation(out=vbuf[:], in_=b3[:, 128:256], func=AF.Copy, scale=rstd), r=["b3", krs], w=[kvb])
                    if not meta:
                        T.seg(3, 0)
                    Pq(lambda e: e.tensor_tensor(out=sq[:], in0=qk[s2][:], in1=qk[s2][:], op=ALU.mult),
                       r=[kqk + "a", kqk + "b"], w=["sq"])
                    khs, khl, khr = "hs%d" % s2, "hl%d" % s2, "hr%d" % s2
                    Vq(lambda e: e.tensor_reduce(out=hs[s2][:], in_=sq[:].rearrange("p (h d) -> p h d", h=10), axis=AX.X, op=ALU.add),
                       r=["sq"], w=[khs])
                    rstd_chain(hs[s2][:], 1.0 / 64, hr[s2][:], hs[s2][:], khs, khs, khr)
                    kqn = "qn%d" % s2
                    Vq(lambda e: e.tensor_tensor(out=qn[s2][:], in0=qk[s2][:].rearrange("p (h d) -> p h d", h=10),
                                                 in1=hr[s2][:].unsqueeze(2).to_broadcast([128, 10, 64]), op=ALU.mult),
                       r=[kqk + "a", kqk + "b", khr], w=[kqn])
                    Pq(lambda e: e.tensor_tensor(out=qn[s2][:], in0=qn[s2][:], in1=qkgain[:], op=ALU.mult),
                       r=[kqn, "qkgain_a", "qkgain_b"], w=[kqn])
                    if not meta:
                        T.seg(4, 0)
                    cs = cosT[:, c, :].unsqueeze(1).to_broadcast([128, 10, 8])
                    sn = sinT[:, c, :].unsqueeze(1).to_broadcast([128, 10, 8])
                    x1 = qn[s2][:, :, 0:8]
                    x2 = qn[s2][:, :, 8:16]
                    kqf = "qf%d" % s2
                    Pq(lambda e: e.tensor_tensor(out=rt[0][:], in0=x1, in1=cs, op=ALU.mult), r=[kqn, "cosT"], w=["rt0"])
                    Pq(lambda e: e.tensor_tensor(out=rt[1][:], in0=x2, in1=sn, op=ALU.mult), r=[kqn, "sinT"], w=["rt1"])
                    Pq(lambda e: e.tensor_tensor(out=qf[s2][:, :, 0:8], in0=rt[0][:], in1=rt[1][:], op=ALU.subtract),
                       r=["rt0", "rt1"], w=[kqf + "a"])
                    Pq(lambda e: e.tensor_tensor(out=rt[2][:], in0=x2, in1=cs, op=ALU.mult), r=[kqn, "cosT"], w=["rt2"])
                    Pq(lambda e: e.tensor_tensor(out=rt[3][:], in0=x1, in1=sn, op=ALU.mult), r=[kqn, "sinT"], w=["rt3"])
                    Pq(lambda e: e.tensor_tensor(out=qf[s2][:, :, 8:16], in0=rt[2][:], in1=rt[3][:], op=ALU.add),
                       r=["rt2", "rt3"], w=[kqf + "b"])
                    Pq(lambda e: e.tensor_copy(out=qf[s2][:, :, 16:64], in_=qn[s2][:, :, 16:64]), r=[kqn], w=[kqf + "c"])
                    qff = qf[s2][:].rearrange("p h d -> p (h d)")
                    if not meta:
                        T.seg(5, 0)

                    def t2(e):
                        for b in range(5):
                            ins = e.transpose(out=tp[:, b * 128:(b + 1) * 128], in_=qff[:, b * 128:(b + 1) * 128], identity=ident[:])
                        return ins
                    Eq(t2, r=[kqf + "a", kqf + "b", kqf + "c", "ident"], w=["tp"])
                    if not meta:
                        Vq(lambda e: e.tensor_copy(out=QT[s2][:].rearrange("p b t -> p (b t)"), in_=tp[:, 0:512]),
                           r=["tp"], w=["QT%d" % s2])
                    Vq(lambda e: e.tensor_copy(out=kbuf[:], in_=tp[:, 512:640]), r=["tp"], w=[kkb])
                    if meta:
                        for g in range(2):
                            Pq(lambda e, g=g: e.memset(Vm[g][:], 0.0), w=["Vm%d" % g])
                            Pq(lambda e, g=g: e.tensor_copy(out=Vm[g][0:16, :], in_=vm_tmp[0:16, g * 64:(g + 1) * 64]),
                               r=["vm_tmp", "Vm%d" % g], w=["Vm%d" % g])

                def stageB(a, gt, first):
                    s2 = a % 2
                    cur, prv = gt % 3, (gt - 1) % 3
                    kuc, kup = "ur%d" % cur, "ur%d" % prv
                    T.seg(0, 1)

                    def pm(e):
                        for gp in range(4):
                            e.matmul(b7[:, gp * 128:(gp + 1) * 128], lhsT=uring[cur][:, gp * 128:(gp + 1) * 128], rhs=Pd[:, gp, :],
                                     start=True, stop=False)
                            if first:
                                ins = e.matmul(b7[:, gp * 128:(gp + 1) * 128], lhsT=um[0:16, gp * 128:(gp + 1) * 128],
                                               rhs=Pm[0:16, gp, :], start=False, stop=True)
                            else:
                                ins = e.matmul(b7[:, gp * 128:(gp + 1) * 128], lhsT=uring[prv][64:128, gp * 128:(gp + 1) * 128],
                                               rhs=Pp[64:128, gp, :], start=False, stop=True)
                        return ins
                    Eq(pm, r=[kuc, "um" if first else kup, "Pd", "Pp", "Pm"], w=["b7"])
                    kmx = "mixT%d" % s2
                    Aq(lambda e: e.activation(out=mixT[s2][:].rearrange("p g t -> p (g t)"), in_=b7[:], func=AF.Copy),
                       r=["b7"], w=[kmx])

                    def pl(e):
                        for gp in range(4):
                            ins = e.matmul(b7[:, gp * 128:(gp + 1) * 128], lhsT=wpl[:, gp, :], rhs=mixT[s2][:, gp, :],
                                           start=True, stop=True)
                        return ins
                    Eq(pl, r=[kmx, "wpl"], w=["b7"])
                    kyp = "ypT%d" % s2
                    Vq(lambda e: e.tensor_tensor(out=ypT[s2][:], in0=b7[:].rearrange("p (g t) -> p g t", g=4),
                                                 in1=ps_t[:].unsqueeze(2).to_broadcast([128, 4, 128]), op=ALU.mult),
                       r=["b7", "ps_t"], w=[kyp])
                    kq = "QT%d" % s2
                    for g in range(2):
                        pr = slice(g * 64, (g + 1) * 64)
                        qrhs = QT[s2][pr, :, :].rearrange("p b t -> p (b t)")
                        T.seg(1 if g == 0 else 4, 1)
                        def sd(e, pr=pr, qrhs=qrhs):
                            e.matmul(b4[:], lhsT=kring[cur][pr, :], rhs=qrhs, start=True, stop=False)
                            return e.matmul(b4[:], lhsT=ident[:], rhs=negm_d[:].rearrange("p h q -> p (h q)"), start=False, stop=True)
                        Eq(sd, r=["kr%d" % cur, kq, "ident", "negm_d"], w=["b4"])
                        if not first:
                            def sp_(e, pr=pr, qrhs=qrhs):
                                e.matmul(b5[:], lhsT=kring[prv][pr, :], rhs=qrhs, start=True, stop=False)
                                return e.matmul(b5[:], lhsT=ident[:], rhs=negm_p[:].rearrange("p h q -> p (h q)"), start=False, stop=True)
                            Eq(sp_, r=["kr%d" % prv, kq, "ident", "negm_p"], w=["b5"])
                        Eq(lambda e, pr=pr, qrhs=qrhs: e.matmul(b6[0:16, :], lhsT=KTm[pr, 0:16], rhs=qrhs, start=True, stop=True),
                           r=["KTm", kq], w=["b6"])
                        kd, kp, km = "PTd%d_%d" % (g, s2), "PTp%d_%d" % (g, s2), "PTm%d_%d" % (g, s2)
                        Aq(lambda e, g=g: e.activation(out=PTd[g][s2][:], in_=b4[:], func=AF.Exp, scale=0.125, bias=negC[:]),
                           r=["b4", "negC"], w=[kd])
                        if not first:
                            Aq(lambda e, g=g: e.activation(out=PTp[g][s2][:], in_=b5[:], func=AF.Exp, scale=0.125, bias=negC[:]),
                               r=["b5", "negC"], w=[kp])
                        Aq(lambda e, g=g: e.activation(out=PTm[g][s2][0:16, :], in_=b6[0:16, :], func=AF.Exp, scale=0.125,
                                                       bias=negC[0:16, :]), r=["b6", "negC"], w=[km])
                        T.seg(3 if g == 0 else 5, 1)

                        def pv(e, g=g, pr=pr):
                            blocks = [(vring[cur][:, pr], ones64[:, :], PTd[g][s2][:, :])]
                            if not first:
                                blocks.append((vring[prv][:, pr], ones64[:, :], PTp[g][s2][:, :]))
                            blocks.append((Vm[g][0:33, :], onesm[0:33, :], PTm[g][s2][0:33, :]))
                            n = len(blocks)
                            for i, (vl, ol, rh) in enumerate(blocks):
                                e.matmul(b1[pr, :], lhsT=vl, rhs=rh, start=(i == 0), stop=(i == n - 1))
                            for i, (vl, ol, rh) in enumerate(blocks):
                                ins = e.matmul(b2[pr, :], lhsT=ol, rhs=rh, start=(i == 0), stop=(i == n - 1))
                            return ins
                        rr = ["vr%d" % cur, kd, km, "Vm%d" % g, "ones64", "onesm"]
                        if not first:
                            rr += ["vr%d" % prv, kp]
                        Eq(pv, r=rr, w=["b1_%d" % g, "b2_%d" % g])
                        krd = "rD%d" % (g)
                        Aq(lambda e, pr=pr: e.activation(out=rD[pr, :], in_=b2[pr, :], func=AF.Ln), r=["b2_%d" % g], w=[krd])
                        Aq(lambda e, pr=pr: e.activation(out=rD[pr, :], in_=rD[pr, :], func=AF.Exp, scale=-1.0), r=[krd], w=[krd])
                        Vq(lambda e, pr=pr: e.tensor_tensor(out=yT[s2][pr, :], in0=b1[pr, :], in1=rD[pr, :], op=ALU.mult),
                           r=["b1_%d" % g, krd], w=["yT%d_%d" % (g, s2)])

                def stageC(a, lt):
                    s2 = a % 2
                    s3 = a % 5
                    kx = "xt%d" % s3
                    kyp = "ypT%d" % s2
                    gtt = rnd * NT + lt
                    T.seg(2, 2)

                    def wo(e):
                        for nh, bank in enumerate((b4, b5)):
                            cols = slice(nh * 512, (nh + 1) * 512)
                            for gp in range(4):
                                e.matmul(bank[:], lhsT=ypT[s2][:, gp, :], rhs=wop[:, gp, cols], start=(gp == 0), stop=False)
                            for hl in range(4):
                                ins = e.matmul(bank[:], lhsT=yT[s2][:, hl * 128:(hl + 1) * 128], rhs=woa[:, hl, cols],
                                               start=False, stop=(hl == 3))
                        return ins
                    Eq(wo, r=[kyp, "yT0_%d" % s2, "yT1_%d" % s2, "wop", "woa"], w=["b4", "b5"])
                    ky = "h1t%d" % s2
                    Vq(lambda e: e.tensor_tensor(out=h1t[s2][:, 0:512], in0=b4[:], in1=xt[s3][:, 0:512], op=ALU.add),
                       r=["b4", kx], w=[ky + "a"])
                    Vq(lambda e: e.tensor_tensor(out=h1t[s2][:, 512:1024], in0=b5[:], in1=xt[s3][:, 512:1024], op=ALU.add),
                       r=["b5", kx], w=[ky + "b"])
                    r0 = gtt * 128
                    T.op("sp", lambda e: e.dma_start(out=out[r0:r0 + 128, :], in_=h1t[s2][:]), r=[ky + "a", ky + "b"],
                         dma="S_h1t%d" % s2)
                    ss = st2[s2][:, 0:1]
                    lnv = st2[s2][:, 1:2]
                    rstd = st2[s2][:, 2:3]
                    kss, kln, krs = "ss2_%d" % s2, "lnv2_%d" % s2, "rstd2_%d" % s2
                    T.seg(3, 2)
                    Aq(lambda e: e.activation(out=junkC[:], in_=h1t[s2][:], func=AF.Square, accum_out=ss),
                       r=[ky + "a", ky + "b"], w=["junkC", kss])
                    rstd_chain(ss, 1.0 / D, rstd, lnv, kss, kln, krs)
                    kmb = "mb%d" % s2
                    Vq(lambda e: e.scalar_tensor_tensor(out=mb[s2][:], in0=h1t[s2][:], scalar=rstd, in1=fgb[:],
                                                        op0=ALU.mult, op1=ALU.mult),
                       r=[ky + "a", ky + "b", krs, "fgb"], w=[kmb])
                    T.op("sp", lambda e: e.dma_start(out=mscr[r0:r0 + 128, :], in_=mb[s2][:]), r=[kmb], dma="S_mb%d" % s2)

                    T.seg(4, 2)

                    def t3(e):
                        for k in range(8):
                            ins = e.transpose(out=tp[:, k * 128:(k + 1) * 128], in_=mb[s2][:, k * 128:(k + 1) * 128], identity=ident[:])
                        return ins
                    Eq(t3, r=[kmb, "ident"], w=["tp"])
                    kmt = "mTt%d" % s2
                    Vq(lambda e: e.tensor_copy(out=mTt[s2][:].rearrange("p k t -> p (k t)"), in_=tp[:]), r=["tp"], w=[kmt])

                    T.seg(5, 2)

                    def rt_(e):
                        for k in range(8):
                            ins = e.matmul(b3[:, 256:276], lhsT=mTt[s2][:, k, :], rhs=wr[:, k, :],
                                           start=(k == 0), stop=(k == 7))
                        return ins
                    Eq(rt_, r=[kmt, "wr"], w=["b3"])
                    R = rs[s2]
                    kr = "rs%d" % s2
                    lgs = R[:, 0:20]
                    gmax, ngmax, gsum, gprob = R[:, 20:21], R[:, 21:22], R[:, 22:23], R[:, 23:24]
                    goh = R[:, 24:28]
                    gexp = R[:, 28:32]
                    tmp = R[:, 32:48]
                    ig = R[:, 48:52]
                    m1, m2, d21, aa = R[:, 52:53], R[:, 53:54], R[:, 54:55], R[:, 55:56]
                    oh1, msk, oh2 = R[:, 56:60], R[:, 60:64], R[:, 64:68]
                    den = R[:, 68:69]
                    w1 = w12[:, gtt, 0:1]
                    w2 = w12[:, gtt, 1:2]
                    kw = "w12_%d" % gtt

                    def RV(fn):
                        Vq(fn, r=[kr], w=[kr])
                    Vq(lambda e: e.tensor_copy(out=lgs, in_=b3[:, 256:276]), r=["b3"], w=[kr])
                    RV(lambda e: e.reduce_max(out=gmax, in_=lgs[:, 0:4], axis=AX.X))
                    RV(lambda e: e.tensor_scalar(out=goh, in0=lgs[:, 0:4], scalar1=gmax, scalar2=None, op0=ALU.is_ge))
                    RV(lambda e: e.tensor_scalar(out=ngmax, in0=gmax, scalar1=-1.0, scalar2=None, op0=ALU.mult))
                    Aq(lambda e: e.activation(out=gexp, in_=lgs[:, 0:4], func=AF.Exp, bias=ngmax, accum_out=gsum), r=[kr], w=[kr])
                    RV(lambda e: e.reciprocal(out=gprob, in_=gsum))
                    RV(lambda e: e.tensor_tensor(out=tmp.rearrange("p (g e) -> p g e", g=4),
                                                 in0=lgs[:, 4:20].rearrange("p (g e) -> p g e", g=4),
                                                 in1=goh.unsqueeze(2).to_broadcast([128, 4, 4]), op=ALU.mult))
                    RV(lambda e: e.tensor_reduce(out=ig, in_=tmp.rearrange("p (g e) -> p e g", g=4), axis=AX.X, op=ALU.add))
                    RV(lambda e: e.reduce_max(out=m1, in_=ig, axis=AX.X))
                    RV(lambda e: e.tensor_scalar(out=oh1, in0=ig, scalar1=m1, scalar2=None, op0=ALU.is_ge))
                    RV(lambda e: e.scalar_tensor_tensor(out=msk, in0=oh1, scalar=-1e30, in1=ig, op0=ALU.mult, op1=ALU.add))
                    RV(lambda e: e.reduce_max(out=m2, in_=msk, axis=AX.X))
                    RV(lambda e: e.tensor_scalar(out=oh2, in0=msk, scalar1=m2, scalar2=None, op0=ALU.is_ge))
                    RV(lambda e: e.tensor_tensor(out=d21, in0=m2, in1=m1, op=ALU.subtract))
                    Aq(lambda e: e.activation(out=aa, in_=d21, func=AF.Exp), r=[kr], w=[kr])
                    RV(lambda e: e.tensor_scalar(out=den, in0=aa, scalar1=1.0, scalar2=None, op0=ALU.add))
                    RV(lambda e: e.reciprocal(out=den, in_=den))
                    Vq(lambda e: e.tensor_tensor(out=w1, in0=den, in1=gprob, op=ALU.mult), r=[kr], w=[kw + "a"])
                    Vq(lambda e: e.tensor_tensor(out=w2, in0=w1, in1=aa, op=ALU.mult), r=[kr, kw + "a"], w=[kw + "b"])
                    ke = "E_%d" % gtt
                    Vq(lambda e: e.tensor_tensor(out=E1all[:, gtt, :].rearrange("p (g e) -> p g e", g=4),
                                                 in0=goh.unsqueeze(2).to_broadcast([128, 4, 4]),
                                                 in1=oh1.unsqueeze(1).to_broadcast([128, 4, 4]), op=ALU.mult), r=[kr], w=[ke + "1"])
                    Vq(lambda e: e.tensor_tensor(out=E2all[:, gtt, :].rearrange("p (g e) -> p g e", g=4),
                                                 in0=goh.unsqueeze(2).to_broadcast([128, 4, 4]),
                                                 in1=oh2.unsqueeze(1).to_broadcast([128, 4, 4]), op=ALU.mult), r=[kr], w=[ke + "2"])
                    Vq(lambda e: e.tensor_tensor(out=Aall[:, gtt, :], in0=E1all[:, gtt, :], in1=E2all[:, gtt, :], op=ALU.add),
                       r=[ke + "1", ke + "2"], w=["A_%d" % gtt])

                PIECES = [("g", 0), ("u", 0), ("g", 1), ("u", 1), ("d", 0), ("d", 1)]

                def conv_load(k):
                    e_, pi = divmod(k, 6)
                    kind, hh = PIECES[pi]
                    s_ = k % 3
                    key = "cstg%d" % s_
                    if kind in ("g", "u"):
                        src = dr["w_gate" if kind == "g" else "w_up"][e_, hh * 512:(hh + 1) * 512, :].rearrange(
                            "(k p) f -> p k f", p=128)
                        dst = cstg[s_][:].rearrange("p (k f) -> p k f", k=4)
                    else:
                        src = dr["w_down"][e_, hh * 128:(hh + 1) * 128, :]
                        dst = cstg[s_][:]
                    T.op("act", lambda e: e.dma_start(out=dst, in_=src), w=[key], dma="D_" + key)

                def conv_cast(k):
                    e_, pi = divmod(k, 6)
                    kind, hh = PIECES[pi]
                    s_ = k % 3
                    key = "cstg%d" % s_
                    par = e_ % 2
                    kwt = "wtile%d" % par
                    if kind in ("g", "u"):
                        off = 0 if kind == "g" else 256
                        wv = wtile[par][:, 0:4096].rearrange("p (k f) -> p k f", k=8)
                        T.op("act", lambda e: e.activation(out=wv[:, hh * 4:(hh + 1) * 4, off:off + 256],
                                                           in_=cstg[s_][:].rearrange("p (k f) -> p k f", k=4), func=AF.Copy),
                             r=[key], w=[kwt + kind + str(hh)])
                    else:
                        T.op("act", lambda e: e.activation(out=wtile[par][:, 4096 + hh * 1024:4096 + (hh + 1) * 1024],
                                                           in_=cstg[s_][:], func=AF.Copy),
                             r=[key], w=[kwt + kind + str(hh)])
                    if pi == 5:
                        T.op("act", lambda e: e.dma_start(out=wscr[e_ * 128:(e_ + 1) * 128, :], in_=wtile[par][:]),
                             r=[kwt + k_ + str(h) for k_ in "gud" for h in range(2)], dma="S_" + kwt)

                cnt = rnd * NT + (1 if True else 0)
                if do_meta:
                    stageA(0, None, 0, meta=True)
                base_gt = rnd * NT

                def xload(lt):
                    s5 = (lt + 1) % 5
                    rows = dr["x"][lt * 128:(lt + 1) * 128, :]
                    T.op("sp", lambda e: e.dma_start(out=xt[s5][:], in_=rows), w=["xt%d" % s5], dma="D_xt%d" % s5)

                xload(0)
                xload(1)
                conv_load(0)
                for step in range(NT + 2):
                    if step + 2 < NT:
                        T.seg(-1, 0)
                        xload(step + 2)
                    if step < NT:
                        lt = step
                        gt = base_gt + lt
                        ti = lt % TPS
                        stageA(step + 1, None, 1 + ti, gt=gt, part="early")
                        stageA(step + 1, None, 1 + ti, gt=gt, part="late")
                    if 1 <= step < NT + 1:
                        lt = step - 1
                        ti = lt % TPS
                        stageB(step, base_gt + lt, first=(ti == 0))
                    if step >= 2:
                        lt = step - 2
                        stageC(step - 1, lt)
                    for zi in range(3 * step, min(3 * step + 3, NSLOT // 128)):
                        T.seg(2 * (zi % 3), 6)
                        T.op("act", lambda e, zi=zi: e.dma_start(out=mslot[zi * 128:(zi + 1) * 128, :], in_=zt[:]),
                             r=["zt"], dma="Z_%d" % (zi % 4))
                    k0 = 3 * step
                    for i_, sl in enumerate((1, 3, 5)):
                        if k0 + i_ < 6 * NE:
                            T.seg(sl, 4)
                            conv_cast(k0 + i_)
                    for kk, sl in ((k0 + 1, 1), (k0 + 2, 3), (k0 + 3, 5)):
                        if kk < 6 * NE:
                            T.seg(sl, 5)
                            conv_load(kk)
                    T.end_step()
                T.flush()

        def dispatch():
            PFX = "dsp"
            with contextlib.ExitStack() as s1:
                def sb1(name, shape, dt=F32):
                    return s1.enter_context(nc.sbuf_tensor("%s_%s" % (PFX, name), list(shape), dt))

                def ps1(name, shape, dt=F32):
                    return s1.enter_context(nc.psum_tensor("%s_%s" % (PFX, name), list(shape), dt))

                rankb = ps1("rankb", [128, NTT * 16])
                cntb = ps1("cntb", [128, 512])
                sc = sb1("sc", [128, 16, 16])
                sci = sb1("sci", [128, 16], I32)
                slotmat = sb1("slotmat", [128, NTT, 16])
                tmpm = sb1("tmpm", [128, NTT, 16])
                slot1f = sb1("slot1f", [128, NTT])
                slot2f = sb1("slot2f", [128, NTT])
                ucmp = sb1("ucmp", [128, NU, 16])
                uio_i = sb1("uio_i", [128, NU, 16], I32)
                uio = sb1("uio", [128, NU, 16])
                euf = sb1("euf", [128, NU])
                pio_i = sb1("pio_i", [128, 1], I32)
                pio = sb1("pio", [128, 1])
                mt = [sb1("mt%d" % i, [128, 1024], BF16) for i in range(8)]

                def Vq(fn, r=(), w=()):
                    T.op("dve", fn, r=r, w=w)

                def Pq(fn, r=(), w=()):
                    T.op("pool", fn, r=r, w=w)

                Pq(lambda e: e.memset(Acum[:, 0, :], 0.0), w=["Acum"])
                for i in range(NTT):
                    Vq(lambda e, i=i: e.tensor_tensor(out=Acum[:, i + 1, :], in0=Acum[:, i, :], in1=Aall[:, i, :], op=ALU.add),
                       r=["Acum"], w=["Acum"])

                def rk(e):
                    for i in range(NTT):
                        e.matmul(rankb[:, i * 16:(i + 1) * 16], lhsT=tri[:], rhs=Aall[:, i, :], start=True, stop=False)
                        ins = e.matmul(rankb[:, i * 16:(i + 1) * 16], lhsT=ones128[:], rhs=Acum[:, i, :], start=False, stop=True)
                    return ins
                T.op("pe", rk, r=["Acum", "tri", "ones128"], w=["rankb"])
                T.op("pe", lambda e: e.matmul(cntb[:, 0:16], lhsT=ones128[:], rhs=Acum[:, NTT, :], start=True, stop=True),
                     r=["Acum", "ones128"], w=["cntb"])
                cnt = sc[:, 0, :]
                nuf = sc[:, 1, :]
                cum = [sc[:, 2, :], sc[:, 3, :]]
                base = sc[:, 4, :]
                k = "sc"

                def SV(fn):
                    Vq(fn, r=[k], w=[k])
                Vq(lambda e: e.tensor_scalar(out=cnt, in0=cntb[:, 0:16], scalar1=255.0, scalar2=1.0 / 256, op0=ALU.add, op1=ALU.mult),
                   r=["cntb"], w=[k])
                SV(lambda e: e.tensor_scalar(out=cnt, in0=cnt, scalar1=-0.499, scalar2=None, op0=ALU.add))
                Vq(lambda e: e.tensor_copy(out=sci[:], in_=cnt), r=[k], w=["sci"])
                Vq(lambda e: e.tensor_copy(out=nuf, in_=sci[:]), r=["sci"], w=[k])
                SV(lambda e: e.tensor_copy(out=cum[0], in_=nuf))
                src = 0
                for sh in (1, 2, 4, 8):
                    a_, b_ = cum[src], cum[1 - src]
                    SV(lambda e, a_=a_, b_=b_, sh=sh: e.tensor_copy(out=b_[:, 0:sh], in_=a_[:, 0:sh]))
                    SV(lambda e, a_=a_, b_=b_, sh=sh: e.tensor_tensor(out=b_[:, sh:16], in0=a_[:, sh:16], in1=a_[:, 0:16 - sh], op=ALU.add))
                    src = 1 - src
                cumu = cum[src]
                SV(lambda e: e.tensor_tensor(out=base, in0=cumu, in1=nuf, op=ALU.subtract))
                SV(lambda e: e.tensor_scalar(out=base, in0=base, scalar1=256.0, scalar2=None, op0=ALU.mult))
                Vq(lambda e: e.tensor_tensor(out=slotmat[:], in0=rankb[:].rearrange("p (i e) -> p i e", e=16),
                                             in1=base.unsqueeze(1).to_broadcast([128, NTT, 16]), op=ALU.add),
                   r=["rankb", k], w=["slotmat"])
                for Eall, sf, si, nm in ((E1all, slot1f, slot1i, "1"), (E2all, slot2f, slot2i, "2")):
                    Vq(lambda e, Eall=Eall: e.tensor_tensor(out=tmpm[:], in0=slotmat[:], in1=Eall[:], op=ALU.mult),
                       r=["slotmat"], w=["tmpm"])
                    Vq(lambda e, sf=sf: e.tensor_reduce(out=sf[:], in_=tmpm[:], axis=AX.X, op=ALU.add), r=["tmpm"], w=["sf" + nm])
                    Vq(lambda e, sf=sf, si=si: e.tensor_copy(out=si[:], in_=sf[:]), r=["sf" + nm], w=["si" + nm])
                Pq(lambda e: e.iota(uio_i[:], pattern=[[1, NU], [0, 16]], base=0, channel_multiplier=0), w=["uio_i"])
                Pq(lambda e: e.iota(pio_i[:], pattern=[[0, 1]], base=0, channel_multiplier=1), w=["pio_i"])
                Vq(lambda e: e.tensor_copy(out=uio[:], in_=uio_i[:]), r=["uio_i"], w=["uio"])
                Vq(lambda e: e.tensor_copy(out=pio[:], in_=pio_i[:]), r=["pio_i"], w=["pio"])
                Vq(lambda e: e.tensor_tensor(out=ucmp[:], in0=cumu.unsqueeze(1).to_broadcast([128, NU, 16]), in1=uio[:], op=ALU.is_le),
                   r=[k, "uio"], w=["ucmp"])
                Vq(lambda e: e.tensor_reduce(out=euf[:], in_=ucmp[:], axis=AX.X, op=ALU.add), r=["ucmp"], w=["euf"])
                Vq(lambda e: e.tensor_scalar(out=euf[:], in0=euf[:], scalar1=128.0, scalar2=pio[:], op0=ALU.mult, op1=ALU.add),
                   r=["euf", "pio"], w=["euf"])
                Vq(lambda e: e.tensor_copy(out=widx[:], in_=euf[:]), r=["euf"], w=["widx"])
                for i in range(NTT):
                    s_ = i % 8
                    km = "mt%d" % s_
                    T.op("sp", lambda e, i=i, s_=s_: e.dma_start(out=mt[s_][:], in_=mscr[i * 128:(i + 1) * 128, :]),
                         w=[km], dma="D_" + km)
                    for si, nm in ((slot1i, "1"), (slot2i, "2")):
                        T.op("pool", lambda e, i=i, s_=s_, si=si: e.indirect_dma_start(
                            out=mslot, out_offset=bass.IndirectOffsetOnAxis(ap=si[:, i:i + 1], axis=0),
                            in_=mt[s_][:, :], in_offset=None),
                            r=[km, "si" + nm], dma="X_%s_%d" % (nm, s_))
                T.flush()

        def experts():
            PFX = "exp"
            with contextlib.ExitStack() as s1:
                def sb1(name, shape, dt=F32):
                    return s1.enter_context(nc.sbuf_tensor("%s_%s" % (PFX, name), list(shape), dt))

                def ps1(name, shape, dt=F32):
                    return s1.enter_context(nc.psum_tensor("%s_%s" % (PFX, name), list(shape), dt))

                wbuf = [sb1("wbuf%d" % i, [128, WROW], BF16) for i in range(3)]
                mtok = [sb1("mtok%d" % i, [128, 2, 1024], BF16) for i in range(4)]
                mTt = [sb1("mTt%d" % i, [128, 8, 128], BF16) for i in range(3)]
                sg = [sb1("sg%d" % i, [128, 256]) for i in range(2)]
                hid = [sb1("hid%d" % i, [128, 256], BF16) for i in range(2)]
                hidT = [sb1("hidT%d" % i, [128, 2, 128], BF16) for i in range(2)]
                ybuf = [sb1("ybuf%d" % i, [128, 1024]) for i in range(3)]
                tp = [ps1("tp%d" % i, [128, 1024], BF16) for i in range(2)]
                hT = ps1("hT", [128, 1024], BF16)
                gu = [ps1("gu%d" % i, [128, 512]) for i in range(2)]
                yps = [ps1("yps%d" % i, [128, 512]) for i in range(3)]
                ycnt = [0]

                def loads(u):
                    p2 = u % 3
                    p3 = u % 4
                    kwb = dict(bounds_check=NE * 128 - 1, oob_is_err=False) if u >= 1 else {}
                    T.op("pool", lambda e, u=u, p2=p2, kwb=kwb: e.indirect_dma_start(
                        out=wbuf[p2][:, :], out_offset=None, in_=wscr,
                        in_offset=bass.IndirectOffsetOnAxis(ap=widx[:, u:u + 1], axis=0), **kwb),
                        w=["wbuf%d" % p2], dma="G_wbuf%d" % p2)
                    T.op("sp", lambda e, u=u, p3=p3: e.dma_start(
                        out=mtok[p3][:], in_=mslot[u * 256:(u + 1) * 256, :].rearrange("(j p) d -> p j d", p=128)),
                        w=["mtok%d" % p3], dma="D_mtok%d" % p3)

                def X1f(t):
                    u, j = divmod(t, 2)
                    p3, t3 = u % 4, t % 3
                    kmk, kmt = "mtok%d" % p3, "mTt%d" % t3
                    tpb, ktp = tp[t % 2], "tp%d" % (t % 2)

                    def tr(e):
                        for k in range(8):
                            ins = e.transpose(out=tpb[:, k * 128:(k + 1) * 128], in_=mtok[p3][:, j, k * 128:(k + 1) * 128],
                                              identity=ident[:])
                        return ins
                    T.op("pe", tr, r=[kmk, "ident"], w=[ktp])
                    T.op("dve", lambda e: e.tensor_copy(out=mTt[t3][:].rearrange("p k t -> p (k t)"), in_=tpb[:]),
                         r=[ktp], w=[kmt])

                def X2f(t):
                    u, j = divmod(t, 2)
                    p2, t2, t3 = u % 3, t % 2, t % 3
                    kw, kmt = "wbuf%d" % p2, "mTt%d" % t3

                    def mgu(e):
                        for k in range(8):
                            ins = e.matmul(gu[t2][:], lhsT=mTt[t3][:, k, :], rhs=wbuf[p2][:, k * 512:(k + 1) * 512],
                                           start=(k == 0), stop=(k == 7))
                        return ins
                    T.op("pe", mgu, r=[kmt, kw], w=["gu%d" % t2])
                    T.op("act", lambda e: e.activation(out=sg[t2][:], in_=gu[t2][:, 0:256], func=AF.Silu),
                         r=["gu%d" % t2], w=["sg%d" % t2])
                    T.op("dve", lambda e: e.tensor_tensor(out=hid[t2][:], in0=gu[t2][:, 256:512], in1=sg[t2][:], op=ALU.mult),
                         r=["gu%d" % t2, "sg%d" % t2], w=["hid%d" % t2])

                def Yf(t):
                    u, j = divmod(t, 2)
                    p2, t2, t3_ = u % 3, t % 2, t % 3
                    kw = "wbuf%d" % p2

                    def th(e):
                        for fc in range(2):
                            ins = e.transpose(out=hT[:, fc * 128:(fc + 1) * 128], in_=hid[t2][:, fc * 128:(fc + 1) * 128],
                                              identity=ident[:])
                        return ins
                    T.op("pe", th, r=["hid%d" % t2, "ident"], w=["hT"])
                    T.op("dve", lambda e: e.tensor_copy(out=hidT[t2][:].rearrange("p f t -> p (f t)"), in_=hT[:, 0:256]),
                         r=["hT"], w=["hidT%d" % t2])
                    T.seg(2, 0)
                    for nh in range(2):
                        yi = ycnt[0] % 3
                        ycnt[0] += 1

                        def dn(e, nh=nh, yi=yi):
                            for fc in range(2):
                                ins = e.matmul(yps[yi][:], lhsT=hidT[t2][:, fc, :],
                                               rhs=wbuf[p2][:, 4096 + fc * 1024 + nh * 512:4096 + fc * 1024 + (nh + 1) * 512],
                                               start=(fc == 0), stop=(fc == 1))
                            return ins
                        T.op("pe", dn, r=["hidT%d" % t2, kw], w=["yps%d" % yi])
                        T.op("act", lambda e, nh=nh, yi=yi: e.activation(out=ybuf[t3_][:, nh * 512:(nh + 1) * 512], in_=yps[yi][:],
                                                                         func=AF.Copy),
                             r=["yps%d" % yi], w=["ybuf%d_%d" % (t3_, nh)])
                    T.op("act", lambda e: e.dma_start(out=yscr[t * 128:(t + 1) * 128, :], in_=ybuf[t3_][:]),
                         r=["ybuf%d_0" % t3_, "ybuf%d_1" % t3_], w=[], dma="S_ybuf%d" % t3_)

                loads(0)
                loads(1)
                loads(2)
                NTL = 2 * NU
                X1f(0)
                X1f(1)
                X2f(0)
                for t in range(NTL):
                    T.seg(0, 0)
                    Yf(t)
                    if t + 1 < NTL:
                        T.seg(1, 1)
                        X2f(t + 1)
                    if t + 2 < NTL:
                        T.seg(3, 2)
                        X1f(t + 2)
                    T.end_step()
                    if t % 2 == 1 and (t - 1) // 2 + 3 < NU:
                        loads((t - 1) // 2 + 3)
                T.flush()

        def combine():
            PFX = "cmb"
            with contextlib.ExitStack() as s1:
                def sb1(name, shape, dt=F32):
                    return s1.enter_context(nc.sbuf_tensor("%s_%s" % (PFX, name), list(shape), dt))

                ya = [sb1("ya%d" % i, [128, 1024]) for i in range(3)]
                yb_ = [sb1("yb%d" % i, [128, 1024]) for i in range(3)]
                hb = [sb1("hb%d" % i, [128, 1024]) for i in range(3)]
                acc = [sb1("acc%d" % i, [128, 1024]) for i in range(3)]
                for i in range(NTT):
                    s_ = i % 3
                    T.op("pool", lambda e, i=i, s_=s_: e.indirect_dma_start(
                        out=ya[s_][:, :], out_offset=None, in_=yscr,
                        in_offset=bass.IndirectOffsetOnAxis(ap=slot1i[:, i:i + 1], axis=0)), w=["ya%d" % s_], dma="G_ya%d" % s_)
                    T.op("pool", lambda e, i=i, s_=s_: e.indirect_dma_start(
                        out=yb_[s_][:, :], out_offset=None, in_=yscr,
                        in_offset=bass.IndirectOffsetOnAxis(ap=slot2i[:, i:i + 1], axis=0)), w=["yb%d" % s_], dma="G_yb%d" % s_)
                    T.op("sp", lambda e, i=i, s_=s_: e.dma_start(out=hb[s_][:], in_=out[i * 128:(i + 1) * 128, :]),
                         w=["hb%d" % s_], dma="D_hb%d" % s_)
                    T.op("dve", lambda e, i=i, s_=s_: e.scalar_tensor_tensor(
                        out=acc[s_][:], in0=ya[s_][:], scalar=w12[:, i, 0:1], in1=hb[s_][:], op0=ALU.mult, op1=ALU.add),
                        r=["ya%d" % s_, "hb%d" % s_], w=["acc%d" % s_])
                    T.op("dve", lambda e, i=i, s_=s_: e.scalar_tensor_tensor(
                        out=acc[s_][:], in0=yb_[s_][:], scalar=w12[:, i, 1:2], in1=acc[s_][:], op0=ALU.mult, op1=ALU.add),
                        r=["yb%d" % s_, "acc%d" % s_], w=["acc%d" % s_])
                    T.op("act", lambda e, i=i, s_=s_: e.dma_start(out=out[i * 128:(i + 1) * 128, :], in_=acc[s_][:]),
                         r=["acc%d" % s_], dma="S_acc%d" % s_)
                T.flush()

        for rnd in range(NR):
            phase1(rnd, do_meta=(rnd == 0))
        dispatch()
        experts()
        combine()
    return nc


_NC_CACHE = {}


def kernel(x, meta_tokens, attn_norm_gain, w_in, w_pool, pool_scale, q_norm_gain, k_norm_gain,
           attn_sinks, w_out, ffn_norm_gain, w_group_router, w_expert_router, w_gate, w_up, w_down):
    f = lambda a: np.ascontiguousarray(np.asarray(a, dtype=np.float32))
    x = f(x)
    B = x.shape[0]
    ncores = 8
    per = B // ncores
    w_in0 = f(w_in)[0]
    perm = []
    for b in range(4):
        for gsel in range(2):
            h = gsel * 4 + b
            perm.extend(range(512 + h * 64, 512 + (h + 1) * 64))
    cols = list(range(512)) + perm + list(range(1024, 1280))
    w_in_p = np.ascontiguousarray(w_in0[:, cols])
    shared = {
        "meta": f(meta_tokens),
        "ag": f(attn_norm_gain).reshape(1, D),
        "w_in": w_in_p,
        "w_pool": f(w_pool)[0],
        "pscale": f(pool_scale).reshape(1, 512),
        "qg": f(q_norm_gain).reshape(1, 64),
        "kg": f(k_norm_gain).reshape(1, 64),
        "sinks": f(attn_sinks).reshape(1, 8),
        "w_out": f(w_out)[0],
        "fg": f(ffn_norm_gain).reshape(1, D),
        "wgr": f(w_group_router)[0],
        "wer": f(w_expert_router)[0],
        "w_gate": f(w_gate)[0],
        "w_up": f(w_up)[0],
        "w_down": f(w_down)[0],
    }
    if "nc" not in _NC_CACHE:
        _NC_CACHE["nc"] = build_nc()
    nc = _NC_CACHE["nc"]
    in_maps = []
    for c in range(ncores):
        m = dict(shared)
        m["x"] = np.ascontiguousarray(x[c * per:(c + 1) * per].reshape(per * SEQ, D))
        in_maps.append(m)
    res = run_bass_kernel_spmd(nc, in_maps, core_ids=list(range(ncores)))
    outs = [np.asarray(r["out"], dtype=np.float32).reshape(per, SEQ, D) for r in res.results]
    return np.concatenate(outs, axis=0)
```
